# Optimizing a Trainium2 kernel written in Bass

```python
import math
import jax, jax.numpy as jnp
from jax import lax
import numpy as np

D_MODEL = 1024
BATCH = 8
SEQ = 4096
DEPTH = 2

HEAD_DIM = D_MODEL // 16
GLA_HEADS = 4
GLA_DK = HEAD_DIM // 2
GLA_DV = HEAD_DIM
GLA_GATE_RANK = 16
GLA_GATE_TAU = 16.0
GLA_CHUNK = 64
CONV_CH = 4 * HEAD_DIM
CONV_GROUPS = 4
CONV_WIDTH = 31
DIFF_HEADS = 4
DIFF_D = HEAD_DIM
Q_BLOCK = 128
D_MIX = GLA_HEADS * GLA_DV + CONV_CH + DIFF_HEADS * 2 * DIFF_D
IN_SPLITS = (GLA_HEADS * GLA_DK, GLA_HEADS * GLA_DK, GLA_HEADS * GLA_DV, GLA_GATE_RANK,
             GLA_HEADS * GLA_DV, 2 * CONV_CH, DIFF_HEADS * 2 * DIFF_D,
             DIFF_HEADS * 2 * DIFF_D, DIFF_HEADS * 2 * DIFF_D)
N_IN = sum(IN_SPLITS)
N_GROUPS = 4
EXPERTS_PER_GROUP = 4
N_EXPERTS = N_GROUPS * EXPERTS_PER_GROUP
TOP_K = 2
D_EXPERT = D_MODEL // 4
EPS = 1e-6

kernel_name = 'hymba_style_hybrid_gla_conv_diffattn_hmoe'


def rms_norm(x, g):
    xf = x.astype(jnp.float32)
    y = xf * lax.rsqrt(jnp.mean(xf * xf, axis=-1, keepdims=True) + EPS)
    return (y * g.astype(jnp.float32)).astype(x.dtype)


def gla_mixer(q, k, v, gate_lr, out_gate, w_gate, b_gate, norm_g):
    B, S, _ = q.shape
    dt = q.dtype
    f32 = jnp.float32
    H, DK, DV, C = GLA_HEADS, GLA_DK, GLA_DV, GLA_CHUNK
    nc = S // C
    log_a = jax.nn.log_sigmoid((gate_lr @ w_gate + b_gate).astype(f32)) / GLA_GATE_TAU

    def to_chunks(t, d):
        return t.astype(f32).reshape(B, nc, C, H, d).transpose(1, 0, 3, 2, 4)

    qc = to_chunks(q, DK) * (DK ** -0.5)
    kc = to_chunks(k, DK)
    vc = to_chunks(v, DV)
    gc = to_chunks(log_a, DK)
    causal = jnp.tril(jnp.ones((C, C), dtype=bool))

    def step(state, inp):
        qi, ki, vi, gi = inp
        b = jnp.cumsum(gi, axis=2)
        o_inter = jnp.einsum('bhik,bhkv->bhiv', qi * jnp.exp(b), state)
        diff = b[:, :, :, None, :] - b[:, :, None, :, :]
        decay = jnp.exp(jnp.where(causal[:, :, None], diff, -jnp.inf))
        scores = jnp.einsum('bhik,bhjk,bhijk->bhij', qi, ki, decay)
        o = o_inter + jnp.einsum('bhij,bhjv->bhiv', scores, vi)
        b_last = b[:, :, -1:, :]
        new_state = (jnp.exp(b_last[:, :, 0, :])[..., None] * state
                     + jnp.einsum('bhjk,bhjv->bhkv', ki * jnp.exp(b_last - b), vi))
        return new_state, o

    s0 = jnp.zeros((B, H, DK, DV), f32)
    _, o = lax.scan(step, s0, (qc, kc, vc, gc))
    o = o.transpose(1, 0, 3, 2, 4).reshape(B, S, H, DV)
    o = rms_norm(o, norm_g).reshape(B, S, H * DV)
    o = o * jax.nn.silu(out_gate.astype(f32))
    return o.astype(dt)


def conv_mixer(u, w, b, gn_g, gn_b):
    dt = u.dtype
    a, gt = jnp.split(u, 2, axis=-1)
    z = a * jax.nn.sigmoid(gt)
    zp = jnp.pad(z, ((0, 0), (CONV_WIDTH - 1, 0), (0, 0)))
    y = lax.conv_general_dilated(zp, w[:, None, :].astype(dt), (1,), 'VALID',
                                 dimension_numbers=('NWC', 'WIO', 'NWC'),
                                 feature_group_count=CONV_CH) + b
    B, S, _ = y.shape
    yf = y.astype(jnp.float32).reshape(B, S, CONV_GROUPS, CONV_CH // CONV_GROUPS)
    mu = jnp.mean(yf, axis=-1, keepdims=True)
    var = jnp.mean(jnp.square(yf - mu), axis=-1, keepdims=True)
    yn = ((yf - mu) * lax.rsqrt(var + EPS)).reshape(B, S, CONV_CH)
    yn = yn * gn_g.astype(jnp.float32) + gn_b.astype(jnp.float32)
    return jax.nn.silu(yn).astype(dt)


def diff_attention(q, k, v, qn_g, kn_g, lq1, lk1, lq2, lk2, subln_g, lambda_init):
    B, S, _ = q.shape
    H, D = DIFF_HEADS, DIFF_D
    f32 = jnp.float32
    qh = rms_norm(q.reshape(B, S, H, 2, D), qn_g).transpose(0, 2, 3, 1, 4)
    kh = rms_norm(k.reshape(B, S, H, 2, D), kn_g).transpose(0, 2, 3, 1, 4)
    vh = v.reshape(B, S, H, 2 * D).transpose(0, 2, 1, 3)
    lam = (jnp.exp(jnp.sum(lq1.astype(f32) * lk1.astype(f32)))
           - jnp.exp(jnp.sum(lq2.astype(f32) * lk2.astype(f32))) + lambda_init)
    scale = D ** -0.5
    k_pos = jnp.arange(S)

    def block(i):
        start = i * Q_BLOCK
        qb = lax.dynamic_slice_in_dim(qh, start, Q_BLOCK, axis=3)
        s = jnp.einsum('bhmqd,bhmkd->bhmqk', qb, kh).astype(f32) * scale
        q_pos = start + jnp.arange(Q_BLOCK)
        s = jnp.where(k_pos[None, :] <= q_pos[:, None], s, -jnp.inf)
        p = jax.nn.softmax(s, axis=-1)
        a = p[:, :, 0] - lam * p[:, :, 1]
        return jnp.einsum('bhqk,bhkv->bhqv', a.astype(vh.dtype), vh)

    o = lax.map(block, jnp.arange(S // Q_BLOCK))
    o = o.transpose(1, 0, 3, 2, 4).reshape(B, S, H, 2 * D)
    o = rms_norm(o, subln_g) * (1.0 - lambda_init)
    return o.reshape(B, S, H * 2 * D)


def hier_moe(h, wg, bg, we, be, w_gate, w_up, w_down):
    B, S, D = h.shape
    f32 = jnp.float32
    t = h.reshape(-1, D)
    T = t.shape[0]
    glog = (t @ wg + bg).astype(f32)
    gprob = jax.nn.softmax(glog, axis=-1)
    g_onehot = jax.nn.one_hot(jnp.argmax(glog, axis=-1), N_GROUPS, dtype=f32)
    p_group = jnp.sum(gprob * g_onehot, axis=-1, keepdims=True)
    elog = (t @ we + be).astype(f32).reshape(T, N_GROUPS, EXPERTS_PER_GROUP)
    elog_sel = jnp.einsum('tge,tg->te', elog, g_onehot)
    eprob = jax.nn.softmax(elog_sel, axis=-1)
    top_p, top_i = lax.top_k(eprob, TOP_K)
    top_p = top_p / jnp.sum(top_p, axis=-1, keepdims=True)
    w_local = jnp.sum(jax.nn.one_hot(top_i, EXPERTS_PER_GROUP, dtype=f32) * top_p[..., None], axis=1)
    gates = (g_onehot[:, :, None] * w_local[:, None, :]).reshape(T, N_EXPERTS) * p_group
    gates = gates.astype(t.dtype)
    y = jnp.zeros_like(t)
    for e in range(N_EXPERTS):
        hid = jax.nn.silu(t @ w_gate[e]) * (t @ w_up[e])
        y = y + gates[:, e:e + 1] * (hid @ w_down[e])
    return y.reshape(B, S, D)


def setup_inputs(seed: int = 0) -> dict:
    key = jax.random.key(seed)
    ks = jax.random.split(key, 32)
    L, D, f32 = DEPTH, D_MODEL, jnp.float32

    def nrm(k, shape, scale):
        return jax.random.normal(k, shape, f32) * scale

    def gain(k, shape):
        return 1.0 + 0.02 * jax.random.normal(k, shape, f32)

    return {
        'x': nrm(ks[0], (BATCH, SEQ, D), 1.0),
        'mix_norm_g': gain(ks[1], (L, D)),
        'w_in': nrm(ks[2], (L, D, N_IN), D ** -0.5),
        'gla_gate_w': nrm(ks[3], (L, GLA_GATE_RANK, GLA_HEADS * GLA_DK), GLA_GATE_RANK ** -0.5),
        'gla_gate_b': nrm(ks[4], (L, GLA_HEADS * GLA_DK), 0.1),
        'gla_norm_g': gain(ks[5], (L, GLA_DV)),
        'conv_w': nrm(ks[6], (L, CONV_WIDTH, CONV_CH), CONV_WIDTH ** -0.5),
        'conv_b': nrm(ks[7], (L, CONV_CH), 0.02),
        'conv_norm_g': gain(ks[8], (L, CONV_CH)),
        'conv_norm_b': nrm(ks[9], (L, CONV_CH), 0.02),
        'diff_qnorm_g': gain(ks[10], (L, DIFF_D)),
        'diff_knorm_g': gain(ks[11], (L, DIFF_D)),
        'diff_lq1': nrm(ks[12], (L, DIFF_D), 0.1),
        'diff_lk1': nrm(ks[13], (L, DIFF_D), 0.1),
        'diff_lq2': nrm(ks[14], (L, DIFF_D), 0.1),
        'diff_lk2': nrm(ks[15], (L, DIFF_D), 0.1),
        'diff_subln_g': gain(ks[16], (L, 2 * DIFF_D)),
        'w_out': nrm(ks[17], (L, D_MIX, D), D_MIX ** -0.5),
        'ffn_norm_g': gain(ks[18], (L, D)),
        'router_group_w': nrm(ks[19], (L, D, N_GROUPS), D ** -0.5),
        'router_group_b': nrm(ks[20], (L, N_GROUPS), 0.01),
        'router_expert_w': nrm(ks[21], (L, D, N_EXPERTS), D ** -0.5),
        'router_expert_b': nrm(ks[22], (L, N_EXPERTS), 0.01),
        'expert_w_gate': nrm(ks[23], (L, N_EXPERTS, D, D_EXPERT), D ** -0.5),
        'expert_w_up': nrm(ks[24], (L, N_EXPERTS, D, D_EXPERT), D ** -0.5),
        'expert_w_down': nrm(ks[25], (L, N_EXPERTS, D_EXPERT, D), D_EXPERT ** -0.5),
    }


def reference(x, mix_norm_g, w_in, gla_gate_w, gla_gate_b, gla_norm_g, conv_w, conv_b,
              conv_norm_g, conv_norm_b, diff_qnorm_g, diff_knorm_g, diff_lq1, diff_lk1,
              diff_lq2, diff_lk2, diff_subln_g, w_out, ffn_norm_g, router_group_w,
              router_group_b, router_expert_w, router_expert_b, expert_w_gate, expert_w_up,
              expert_w_down):
    offsets = [int(o) for o in np.cumsum(IN_SPLITS)[:-1]]
    for l in range(DEPTH):
        hn = rms_norm(x, mix_norm_g[l])
        proj = hn @ w_in[l]
        gq, gk, gv, glr, gog, cu, dq, dk, dv = jnp.split(proj, offsets, axis=-1)
        o_gla = gla_mixer(gq, gk, gv, glr, gog, gla_gate_w[l], gla_gate_b[l], gla_norm_g[l])
        o_conv = conv_mixer(cu, conv_w[l], conv_b[l], conv_norm_g[l], conv_norm_b[l])
        lambda_init = 0.8 - 0.6 * math.exp(-0.3 * l)
        o_diff = diff_attention(dq, dk, dv, diff_qnorm_g[l], diff_knorm_g[l], diff_lq1[l],
                                diff_lk1[l], diff_lq2[l], diff_lk2[l], diff_subln_g[l], lambda_init)
        mix = jnp.concatenate([o_gla, o_conv, o_diff], axis=-1)
        x = x + mix @ w_out[l]
        x = x + hier_moe(rms_norm(x, ffn_norm_g[l]), router_group_w[l], router_group_b[l],
                         router_expert_w[l], router_expert_b[l], expert_w_gate[l],
                         expert_w_up[l], expert_w_down[l])
    return x
```

```python
import contextlib
import math
import numpy as np
import concourse.bass as bass
import concourse.mybir as mybir
from concourse.bass_utils import run_bass_kernel_spmd

F32 = mybir.dt.float32
BF16 = mybir.dt.bfloat16
ALU = mybir.AluOpType
AF = mybir.ActivationFunctionType
AX = mybir.AxisListType

COMPUTE = ("pe", "act", "dve", "pool")
STREAMS = ("pe", "act", "dve", "pool", "sp")


class _Op:
    __slots__ = ("stream", "fn", "reads", "writes", "is_dma", "idx", "sidx", "signal",
                 "deps", "count", "dsem", "dval", "dprev", "tag")

    def __init__(self, stream, fn, reads, writes, is_dma):
        self.stream = stream
        self.fn = fn
        self.reads = reads
        self.writes = writes
        self.is_dma = is_dma
        self.signal = is_dma
        self.deps = []
        self.count = None
        self.dsem = None
        self.dval = None
        self.dprev = None


def _flat(keys):
    out = []
    for k in keys:
        if isinstance(k, list):
            out.extend(_flat(k))
        else:
            out.append(k)
    return out


class Sched:
    def __init__(self, nc, n_dma_sems=8):
        self.nc = nc
        self.ops = []
        self.n_dma_sems = n_dma_sems
        self.tag = "init"
        self.eng = {"pe": nc.tensor, "act": nc.scalar, "dve": nc.vector, "pool": nc.gpsimd,
                    "sp": nc.sync}

    def add(self, stream, fn, reads=(), writes=(), dma=False):
        op = _Op(stream, fn, tuple(_flat(reads)), tuple(_flat(writes)), dma)
        op.tag = self.tag
        op.idx = len(self.ops)
        self.ops.append(op)
        return op

    def op(self, stream, method, *args, r=(), w=(), **kw):
        return self.add(stream, lambda e: getattr(e, method)(*args, **kw), r, w)

    def dma(self, stream, out, in_, r=(), w=(), **kw):
        return self.add(stream, lambda e: e.dma_start(out=out, in_=in_, **kw), r, w, dma=True)

    def mm(self, out, lhsT, rhs, start, stop, r=(), w=(), tp=None, skip=False):
        kw = {}
        if tp is not None:
            kw["tile_position"] = tp
        if skip:
            kw["skip_group_check"] = True
        fn = lambda e: e.matmul(out, lhsT, rhs, start=start, stop=stop, **kw)
        op = self.add("pe", fn, r, w)
        op.tag = (op.tag, "%d*%d*%d" % (lhsT.shape[0], lhsT.shape[1], rhs.shape[-1] if len(rhs.shape) == 2
                                          else int(np.prod(rhs.shape[1:]))))
        return op

    def finalize(self, stack):
        nc = self.nc
        ops = self.ops
        last_w = {}
        readers = {}
        for op in ops:
            deps = set()
            for k in op.reads:
                w = last_w.get(k)
                if w is not None:
                    deps.add((w, "raw"))
            for k in op.writes:
                w = last_w.get(k)
                if w is not None:
                    deps.add((w, "waw"))
                for r in readers.get(k, ()):
                    deps.add((r, "war"))
            for k in op.reads:
                readers.setdefault(k, []).append(op.idx)
            for k in op.writes:
                last_w[k] = op.idx
                readers[k] = []
            best = {}
            dma_deps = set()
            for (d, kind) in deps:
                if d == op.idx:
                    continue
                p = ops[d]
                if p.is_dma:
                    dma_deps.add(d)
                    continue
                if p.stream == op.stream and not op.is_dma:
                    if p.stream == "pe":
                        continue
                    if kind != "raw":
                        continue
                if best.get(p.stream, -1) < d:
                    best[p.stream] = d
            op.deps = sorted(best.values()) + sorted(dma_deps)
        known = {s: {} for s in STREAMS}
        cnt = {s: 0 for s in STREAMS}
        for op in ops:
            cnt[op.stream] += 1
            op.sidx = cnt[op.stream]
        for op in ops:
            kept = []
            kn = known[op.stream]
            for d in op.deps:
                p = ops[d]
                if p.is_dma:
                    key = ("dma", d)
                    if key in kn:
                        continue
                    kn[key] = True
                    kept.append(d)
                else:
                    if kn.get(p.stream, 0) >= p.sidx:
                        continue
                    kn[p.stream] = p.sidx
                    kept.append(d)
                    p.signal = True
            op.deps = kept
        sems = {}
        for s in COMPUTE:
            sems[s] = stack.enter_context(nc.semaphore("sem_" + s))
        c = {s: 0 for s in COMPUTE}
        dma_pool = {}
        dma_state = {}
        for op in ops:
            if op.is_dma:
                pool = dma_pool.get(op.stream)
                if pool is None:
                    pool = [stack.enter_context(nc.semaphore("dsem_%s_%d" % (op.stream, i)))
                            for i in range(self.n_dma_sems)]
                    dma_pool[op.stream] = pool
                    dma_state[op.stream] = [0, [0] * self.n_dma_sems]
                st = dma_state[op.stream]
                i = st[0] % self.n_dma_sems
                st[0] += 1
                op.dsem = pool[i]
                op.dprev = st[1][i]
                st[1][i] += 16
                op.dval = st[1][i]
            elif op.signal:
                c[op.stream] += 1
                op.count = c[op.stream]
        self.max_counts = dict(c)
        self.n_ops = dict(cnt)
        assert max(c.values()) < 60000, c
        for op in ops:
            e = self.eng[op.stream]
            for d in op.deps:
                p = ops[d]
                if p.is_dma:
                    e.wait_ge(p.dsem, p.dval)
                else:
                    e.wait_ge(sems[p.stream], p.count)
            if op.is_dma:
                if op.dprev > 0:
                    e.wait_ge(op.dsem, op.dprev)
                ins = op.fn(e)
                ins.then_inc(op.dsem, 16)
            else:
                if op.fn is None:
                    continue
                ins = op.fn(e)
                if op.signal:
                    ins.then_inc(sems[op.stream], 1)
        return self


D = 1024
T = 512
NIN = 2832
EPS = 1e-6
NE = 16
FM_COLS = [0, 128, 784, 912, 1040, 1168] + [1296 + 128 * h for h in range(4)] + \
          [1808 + 128 * h for h in range(4)]
FM_GQ, FM_GK, FM_A0, FM_A1, FM_G0, FM_G1, FM_DQ, FM_DK = 0, 1, 2, 3, 4, 5, 6, 10
TM_COLS = [256, 528, 2320, 2576]
PC_MIXG, PC_FFNG, PC_GB, PC_CB, PC_CG, PC_CBETA, PC_QG, PC_KG, PC_SUB, PC_CW = 0, 8, 16, 17, 19, 21, 23, 24, 25, 26
NPC = 26 + 62
PR_G8, PR_RB, PR_LQ = 0, 256, 336
NPR = 336 + 256


def lambda_init(l):
    return 0.8 - 0.6 * math.exp(-0.3 * l)


class _Stop(Exception):
    pass


def build(SEQ, DEPTH, debug=(), stop=None):
    NB = SEQ // T
    NT = SEQ // 128
    nc = bass.Bass("TRN2", target_bir_lowering=False)
    L = DEPTH

    def din(name, shape, dt=F32):
        return nc.dram_tensor(name, shape, dt, kind="ExternalInput").ap()

    def dint(name, shape, dt):
        return nc.dram_tensor(name, shape, dt, kind="Internal").ap()

    xT_d = din("xT", [D, SEQ])
    w_fm = din("w_fm", [L, 14 * 128, 1024])
    w_glr = din("w_glr", [L, 128, 128])
    w_tm = din("w_tm", [L, 4 * 128, 2048])
    w_out = din("w_out", [L, 8 * 128, 1024])
    w_gu = din("w_gu", [L, 32 * 128, 2048])
    w_d = din("w_d", [L, 16 * 128, 2048])
    w_r = din("w_r", [L, 128, 160])
    w_gate = din("w_gate", [L, 16, 128])
    pcol = din("pcol", [L, 128, NPC])
    prow = din("prow", [L, 128, NPR])
    w_fm_b = dint("w_fm_b", [L, 14 * 128, 1024], BF16)
    w_glr_b = dint("w_glr_b", [L, 128, 128], BF16)
    w_tm_b = dint("w_tm_b", [L, 4 * 128, 2048], BF16)
    w_out_b = dint("w_out_b", [L, 8 * 128, 1024], BF16)
    w_gu_b = dint("w_gu_b", [L, 32 * 128, 2048], BF16)
    w_d_b = dint("w_d_b", [L, 16 * 128, 2048], BF16)
    xmid = dint("xmid", [D, SEQ], F32)
    yT_d = nc.dram_tensor("yT", [D, SEQ], F32, kind="ExternalOutput").ap()
    dbg_outs = {}

    st = contextlib.ExitStack()
    with st:
        S = Sched(nc)

        def sb(name, shape, dt):
            return st.enter_context(nc.sbuf_tensor(name, shape, dt))

        psall = st.enter_context(nc.psum_tensor("psall", [128, 4096], F32))

        class _Bank:
            def __init__(self, b):
                self.b = b

            def __getitem__(self, key):
                return psall[:, self.b * 512:(self.b + 1) * 512][key]

        ps = [_Bank(b) for b in range(8)]

        def pk(b, lo=0, hi=512):
            return [("ps", b)]

        def psbf(b):
            return psall[:, b * 512:(b + 1) * 512].bitcast(BF16)

        kT = sb("kT", [128, 4, SEQ], BF16)
        vS = sb("vS", [128, NT, 512], BF16)
        NSLOT = 10
        xT = sb("xT_sb", [128, NSLOT, T], F32)
        hn = sb("hn", [128, 8, T], BF16)
        ident = sb("ident", [128, 128], BF16)
        tri = sb("tri", [128, 128], BF16)
        ones1 = sb("ones1", [128, 128], BF16)
        onesD = sb("onesD", [128, 128], BF16)
        ones128 = sb("ones128", [128, 128], BF16)
        blk64 = sb("blk64", [128, 128], BF16)
        rmask = sb("rmask", [128, T], F32)
        cst = sb("cst", [128, 4], F32)
        pcol_sb = sb("pcol_sb", [128, NPC], F32)
        prow_sb = sb("prow_sb", [128, NPR], F32)
        wr_sb = sb("wr_sb", [128, 160], F32)
        wgate_bf = sb("wgate_bf", [16, 128], BF16)
        wgate_f = sb("wgate_f", [16, 128], F32)
        wglr_sb = sb("wglr_sb", [128, 128], BF16)
        der = sb("der", [128, 8], F32)
        g8 = sb("g8", [128, 256], F32)
        S32 = sb("S32", [128, 64], F32)
        Sblk = sb("Sblk", [128, 256], BF16)
        Qblk = sb("Qblk", [128, 4, 4, 128], BF16)
        zb = sb("zb", [128, 2, 30 + T], BF16)
        lamt = sb("lamt", [128, 128], F32)
        NFM, NTM, NGU, NWD = 4, 2, 4, 3
        wfm_r = [sb("wfm_r%d" % i, [128, 8, 128], BF16) for i in range(NFM)]
        wtm_r = [sb("wtm_r%d" % i, [128, 8, 256], BF16) for i in range(NTM)]
        wgu_r = [sb("wgu_r%d" % i, [128, 2, 8, 128], BF16) for i in range(NGU)]
        wd_r = [sb("wd_r%d" % i, [128, 16, 128], BF16) for i in range(NWD)]
        ARENA = 14080 + 128
        arena = sb("arena", [128, ARENA], F32)

        GR = 128

        class Buf:
            def __init__(self, ap, base, dt, cols):
                self.ap, self.base, self.dt, self.cols = ap, base, dt, cols
                self.per = 2 if dt == BF16 else 1

            def k(self, lo=0, hi=None):
                hi = self.cols if hi is None else hi
                a = self.base + lo // self.per
                b = self.base + (hi + self.per - 1) // self.per
                return [("ar", g) for g in range(a // GR, (b + GR - 1) // GR)]

        class Arena:
            def __init__(self):
                self.off = 0

            def _take(self, c32):
                c32 = ((c32 + GR - 1) // GR) * GR
                a = self.off
                self.off += c32
                assert self.off <= ARENA, self.off
                return a, c32

            def f32(self, cols):
                a, c32 = self._take(cols)
                return Buf(arena[:, a:a + cols], a, F32, cols)

            def bf16(self, cols):
                a, c32 = self._take((cols + 1) // 2)
                return Buf(arena[:, a:a + (cols + 1) // 2].bitcast(BF16), a, BF16, cols)

        A = Arena()
        mixT_b = A.bf16(8 * T)
        mixT = mixT_b.ap.rearrange("p (c t) -> p c t", t=T)
        gq_sb = A.f32(T)
        gk_sb = A.f32(T)
        glr_bf = A.bf16(T)
        gv_b = A.bf16(4 * 256)
        gv_sb = gv_b.ap.rearrange("p (c t) -> p c t", t=256)
        gsg_b = A.f32(4 * 256)
        gsg = gsg_b.ap.rearrange("p (c t) -> p c t", t=256)
        qn_b = A.bf16(4 * T)
        qn = qn_b.ap.rearrange("p (c t) -> p c t", t=T)
        tA = [A.f32(T) for _ in range(6)]
        tB = [A.bf16(T) for _ in range(4)]
        PTp = [A.bf16(2 * T) for _ in range(2)]
        qt = A.bf16(T)
        kt = A.bf16(T)
        kt_b = A.bf16(4 * 128)
        kt_tok = kt_b.ap.rearrange("p (c t) -> p c t", t=128)
        og2 = [A.bf16(256) for _ in range(2)]
        small = A.f32(128)
        dg_b = A.bf16(31 * 128)
        dg = dg_b.ap.rearrange("p (c t) -> p c t", t=128)
        mixer_top = A.off
        M = Arena()
        actp_b = M.bf16(32 * T)
        actp = actp_b.ap.rearrange("p (c t) -> p c t", t=T)
        xn32 = [M.f32(T) for _ in range(2)]
        sgb = [M.f32(T) for _ in range(2)]
        hb = [M.f32(T) for _ in range(2)]
        De = [M.bf16(4 * 128) for _ in range(2)]
        rt_b = M.f32(640)
        rt = rt_b.ap
        moe_top = M.off

        def dbg(name, ap, shape, dt=F32, r=()):
            if name not in debug:
                return
            t = nc.dram_tensor("dbg_" + name, list(shape), dt, kind="ExternalOutput").ap()
            dbg_outs[name] = t
            S.dma("sp", t, ap, r=list(r), w=[("dbg", name)])

        S.op("pool", "memset", ones1[:], 1.0, w=["c_ones1"])
        S.op("pool", "memset", onesD[:], 1.0 / 1024.0, w=["c_onesD"])
        S.op("pool", "memset", ones128[:], 1.0 / 128.0, w=["c_ones128"])
        S.op("pool", "memset", ident[:], 1.0, w=["c_ident"])
        S.op("pool", "affine_select", ident[:], ident[:], [[-1, 128]], ALU.is_equal, 0.0, base=0,
             channel_multiplier=1, r=["c_ident"], w=["c_ident"])
        S.op("pool", "memset", tri[:], 1.0, w=["c_tri"])
        S.op("pool", "affine_select", tri[:], tri[:], [[1, 128]], ALU.is_ge, 0.0, base=0,
             channel_multiplier=-1, r=["c_tri"], w=["c_tri"])
        S.op("pool", "memset", blk64[:], 0.0, w=["c_blk64"])
        S.op("pool", "memset", blk64[0:64, 0:64], 1.0 / 64.0, r=["c_blk64"], w=["c_blk64"])
        S.op("pool", "memset", blk64[64:128, 64:128], 1.0 / 64.0, r=["c_blk64"], w=["c_blk64"])
        S.op("pool", "memset", Qblk[:].rearrange("p a b c -> p (a b c)"), 0.0, w=["Qblk"])
        S.op("pool", "memset", rmask[:], 1.0, w=["c_rmask"])
        S.op("pool", "memset", rmask[:].rearrange("p (c t) -> p c t", t=128)[:, :, 0:1], 0.0,
             r=["c_rmask"], w=["c_rmask"])

        S.op("pool", "memset", cst[:, 0:1], EPS, w=["c_eps"])
        S.op("pool", "memset", cst[:, 1:2], 64.0 * EPS, r=["c_eps"], w=["c_eps"])
        S.op("pool", "memset", cst[:, 2:3], 1.0, r=["c_eps"], w=["c_eps"])
        S.op("pool", "memset", cst[:, 3:4], 0.0, r=["c_eps"], w=["c_eps"])
        epsc = {EPS: cst[:, 0:1], 64.0 * EPS: cst[:, 1:2], 1.0: cst[:, 2:3]}

        def flat128(ap):
            return ap.rearrange("(p a) c -> p (a c)", p=128)

        for c_ in range(8):
            S.dma("sp", xT[:, c_, :], xT_d[c_ * 128:(c_ + 1) * 128, 0:T], w=[("xT", c_)])

        def emit_casts(l):
            for j in range(14):
                S.dma("pool", w_fm_b[l, j * 128:(j + 1) * 128, :], w_fm[l, j * 128:(j + 1) * 128, :],
                      r=[("xT", c_) for c_ in range(8)] if (l == 0 and j == 2) else [],
                      w=[("c_wfm", l, j)])
                if j == 0:
                    S.dma("pool", w_glr_b[l], w_glr[l], w=[("c_wglr", l)])
            for j in range(4):
                S.dma("pool", w_tm_b[l, j * 128:(j + 1) * 128, :], w_tm[l, j * 128:(j + 1) * 128, :],
                      w=[("c_wtm", l, j)])
            for j in range(8):
                S.dma("pool", w_out_b[l, j * 128:(j + 1) * 128, :], w_out[l, j * 128:(j + 1) * 128, :],
                      w=[("c_wout", l, j)])
            for g in range(4):
                S.dma("pool", flat128(w_gu_b[l, g * 1024:(g + 1) * 1024]),
                      flat128(w_gu[l, g * 1024:(g + 1) * 1024]),
                      r=[("c_wfm", l, 13), ("c_wtm", l, 3), ("c_wout", l, 7)] if g == 0
                      else [("c_wgu", l, g - 1)],
                      w=[("c_wgu", l, g)])
            for g in range(2):
                S.dma("pool", flat128(w_d_b[l, g * 1024:(g + 1) * 1024]),
                      flat128(w_d[l, g * 1024:(g + 1) * 1024]),
                      r=[("c_wgu", l, 1)] if g == 0 else [("c_wd", l, 0), ("c_wgu", l, 3)],
                      w=[("c_wd", l, g)])

        emit_casts(0)

        class Ring:
            def __init__(self, name, bufs, items, loader, hold=1):
                self.name, self.bufs, self.items, self.loader = name, bufs, items, loader
                self.hold = hold
                self.n_loaded = 0
                self.n_used = 0

            def key(self, i):
                return (self.name, i % len(self.bufs))

            def prefetch(self, upto):
                upto = min(upto, len(self.items))
                while self.n_loaded < upto:
                    i = self.n_loaded
                    self.loader(self.bufs[i % len(self.bufs)], self.items[i], self.key(i))
                    self.n_loaded += 1

            def next(self):
                i = self.n_used
                self.prefetch(i + len(self.bufs) - (self.hold - 1))
                self.n_used += 1
                return self.bufs[i % len(self.bufs)], self.key(i)

        DOWN_ORDER = (2, 3, 4, 5, 6, 7, 0, 1)
        fm_items = []
        tm_items = []
        gu_items = []
        wd_items = []
        for l in range(L):
            for I in range(NB):
                for j in range(14):
                    fm_items.append(("fm", l, j))
                for j in range(8):
                    fm_items.append(("out", l, j))
                for g in range(4):
                    tm_items.append((l, g))
                for e in range(NE):
                    for gu in range(2):
                        gu_items.append((l, e, gu))
                for j in DOWN_ORDER:
                    for half in range(2):
                        wd_items.append((l, j, half))

        def load_fm(buf, it, key):
            kind, l, j = it
            if kind == "fm":
                S.dma("sp", buf[:].rearrange("p a b -> p (a b)"), w_fm_b[l, j * 128:(j + 1) * 128, :],
                      r=[("c_wfm", l, j)], w=[key])
            else:
                S.dma("sp", buf[:].rearrange("p a b -> p (a b)"), w_out_b[l, j * 128:(j + 1) * 128, :],
                      r=[("c_wout", l, j)], w=[key])

        def load_tm(buf, it, key):
            l, g = it
            S.dma("sp", buf[:].rearrange("p a b -> p (a b)"), w_tm_b[l, g * 128:(g + 1) * 128, :],
                  r=[("c_wtm", l, g)], w=[key])

        def load_gu(buf, it, key):
            l, e, gu = it
            row = (e * 2 + gu) * 128
            S.dma("sp", buf[:].rearrange("p a b c -> p (a b c)"), w_gu_b[l, row:row + 128, :],
                  r=[("c_wgu", l, e // 4)], w=[key])

        def load_wd(buf, it, key):
            l, j, half = it
            row = (j * 2 + half) * 128
            S.dma("sp", buf[:].rearrange("p a b -> p (a b)"), w_d_b[l, row:row + 128, :],
                  r=[("c_wd", l, j // 4)], w=[key])

        R_fm = Ring("r_fm", wfm_r, fm_items, load_fm)
        R_tm = Ring("r_tm", wtm_r, tm_items, load_tm)
        R_gu = Ring("r_gu", wgu_r, gu_items, load_gu, hold=2)
        R_wd = Ring("r_wd", wd_r, wd_items, load_wd)

        bank_rr = [0]

        def nextbank(choices):
            b = choices[bank_rr[0] % len(choices)]
            bank_rr[0] += 1
            return b

        ev = [0]

        def alt(engs=("dve", "pool")):
            ev[0] += 1
            return engs[ev[0] % len(engs)]

        def rstd_act(dst, dkeys, src, skeys, eps):
            S.op("act", "activation", dst, src, AF.Ln, bias=epsc[eps], r=skeys + ["c_eps"], w=dkeys)
            S.op("act", "activation", dst, dst, AF.Exp, scale=-0.5, r=dkeys, w=dkeys)

        def recip1p(dst, dkeys):
            S.op("act", "activation", dst, dst, AF.Ln, bias=epsc[1.0], r=dkeys + ["c_eps"], w=dkeys)
            S.op("act", "activation", dst, dst, AF.Exp, scale=-1.0, r=dkeys, w=dkeys)

        def ck(name):
            S.tag = "post_" + name
            if stop == name:
                raise _Stop()

        def tr_op(o, i_, r, w):
            S.add("pe", lambda e: e.transpose(o, i_, ident[:]), r, w)

        try:
          ck("casts")
          for l in range(L):
            src = xT_d if l == 0 else xmid
            dst = yT_d if l == L - 1 else xmid
            li = lambda_init(l)
            S.dma("sp", pcol_sb[:], pcol[l], w=["pcol"])
            S.dma("sp", prow_sb[:], prow[l], w=["prow"])
            S.dma("sp", wr_sb[:], w_r[l], w=["wr"])
            S.dma("sp", wgate_f[:], w_gate[l], w=["wgate_f"])
            S.op("dve", "tensor_copy", wgate_bf[:], wgate_f[:], r=["wgate_f"], w=["wgate"])
            S.dma("sp", wglr_sb[:], w_glr_b[l], r=[("c_wglr", l)], w=["wglr"])
            S.op("dve", "tensor_scalar", der[:, 0:1], pcol_sb[:, PC_GB:PC_GB + 1], -1.0, None, ALU.mult,
                 r=["pcol"], w=["der"])
            S.op("dve", "tensor_scalar", der[:, 1:2], pcol_sb[:, PC_SUB:PC_SUB + 1], 1.0 - li, None,
                 ALU.mult, r=["pcol"], w=["der"])
            S.op("dve", "tensor_scalar", g8[:], prow_sb[:, PR_G8:PR_G8 + 256], 8.0, None, ALU.mult,
                 r=["prow"], w=["g8"])
            S.op("dve", "tensor_tensor", lamt[:, 0:64], prow_sb[:, PR_LQ:PR_LQ + 64],
                 prow_sb[:, PR_LQ + 64:PR_LQ + 128], ALU.mult, r=["prow"], w=["lamt"])
            S.op("dve", "tensor_tensor", lamt[:, 64:128], prow_sb[:, PR_LQ + 128:PR_LQ + 192],
                 prow_sb[:, PR_LQ + 192:PR_LQ + 256], ALU.mult, r=["prow", "lamt"], w=["lamt"])
            S.op("dve", "tensor_reduce", der[:, 4:6], lamt[:].rearrange("p (a b) -> p a b", b=64),
                 AX.X, ALU.add, r=["lamt"], w=["der"])
            S.op("act", "activation", der[:, 6:8], der[:, 4:6], AF.Exp, r=["der"], w=["der"])
            S.op("dve", "tensor_tensor", der[:, 2:3], der[:, 7:8], der[:, 6:7], ALU.subtract,
                 r=["der"], w=["der"])
            S.op("dve", "tensor_scalar", der[:, 2:3], der[:, 2:3], -li, None, ALU.add,
                 r=["der"], w=["der"])
            S.op("dve", "memset", S32[:], 0.0, w=["S32"])
            S.op("dve", "memset", Sblk[:], 0.0, w=["Sblk"])
            S.op("dve", "memset", zb[:, :, 0:30], 0.0, w=[("zb", 0), ("zb", 1)])

            ck("params")
            for I in range(NB):
                t0 = I * T
                gb = l * NB + I

                def xslot(j, g=None):
                    return (j + 2 * (gb if g is None else g)) % NSLOT

                def xc(j, g=None):
                    return xT[:, xslot(j, g), :]

                def xk(j, g=None):
                    return ("xT", xslot(j, g))

                if l + 1 < L and I == min(1, NB - 1):
                    emit_casts(l + 1)

                def norm_sq(kc):
                    S.op("act", "activation", hn[:, kc, :], xc(kc), AF.Square,
                         r=[xk(kc)], w=[("hn", kc)])

                def norm_mm(kc, bank, first, last):
                    S.mm(ps[bank][:], onesD[:], hn[:, kc, :], first, last,
                         r=[("hn", kc), "c_onesD"], w=pk(bank))

                def rmsnorm(gbase, bank, router, stats_done=False):
                    order = (6, 7, 0, 1, 2, 3, 4, 5)
                    if not stats_done:
                        for kc in order:
                            S.op("act", "activation", hn[:, kc, :], xc(kc), AF.Square,
                                 r=[xk(kc)], w=[("hn", kc)])
                        for ki, kc in enumerate(order):
                            S.mm(ps[bank][:], onesD[:], hn[:, kc, :], ki == 0, ki == 7,
                                 r=[("hn", kc), "c_onesD"], w=pk(bank))
                    rstd = tA[0]
                    rstd_act(rstd.ap, rstd.k(), ps[bank][:], pk(bank), EPS)
                    for kc in range(8):
                        gc = pcol_sb[:, gbase + kc:gbase + kc + 1]
                        if not router:
                            S.op("dve", "scalar_tensor_tensor", hn[:, kc, :], xc(kc), gc, rstd.ap,
                                 ALU.mult, ALU.mult, r=[xk(kc), rstd.k(), "pcol"], w=[("hn", kc)])
                        else:
                            xb = xn32[kc % 2]
                            S.op("dve", "scalar_tensor_tensor", xb.ap, xc(kc), gc, rstd.ap,
                                 ALU.mult, ALU.mult, r=[xk(kc), rstd.k(), "pcol"], w=xb.k())
                            S.op("act", "copy", hn[:, kc, :], xb.ap, r=xb.k(), w=[("hn", kc)])
                            for tt in range(4):
                                S.mm(ps[3][:, tt * 20:(tt + 1) * 20], xb.ap[:, tt * 128:(tt + 1) * 128],
                                     wr_sb[:, kc * 20:(kc + 1) * 20], kc == 0 and tt == 0,
                                     kc == 7 and tt == 3, r=[xb.k(), "wr"], w=pk(3, 0, 128), skip=True)

                ck("load")
                rmsnorm(PC_MIXG, 0, False)
                ck("norm1")
                dbg("hn%d_%d" % (l, I), hn[:], [128, 8, T], BF16, r=[("hn", c) for c in range(8)])

                def fm_chunk():
                    wbuf, wkey = R_fm.next()
                    b = nextbank([0, 1, 2, 3, 4, 7])
                    for kc in range(8):
                        S.mm(ps[b][:], wbuf[:, kc, :], hn[:, kc, :], kc == 0, kc == 7,
                             r=[wkey, ("hn", kc)], w=pk(b))
                    return b

                b = fm_chunk()
                S.op("act", "copy", gq_sb.ap, ps[b][:], r=pk(b), w=gq_sb.k())
                b = fm_chunk()
                S.op("act", "copy", gk_sb.ap, ps[b][:], r=pk(b), w=gk_sb.k())
                ba = [fm_chunk(), fm_chunk()]
                for cc in range(2):
                    bg = fm_chunk()
                    tt_ = tA[1 + cc]
                    S.op("act", "activation", tt_.ap, ps[bg][:], AF.Exp, scale=-1.0, r=pk(bg), w=tt_.k())
                    recip1p(tt_.ap, tt_.k())
                    S.op("dve", "tensor_tensor", zb[:, cc, 30:30 + T], ps[ba[cc]][:], tt_.ap,
                         ALU.mult, r=pk(ba[cc]) + tt_.k(), w=[("zb", cc)])
                pend = None

                def qk_finish(p):
                    b, is_k, h, sqb = p
                    b2 = nextbank([5, 6])
                    S.mm(ps[b2][:], blk64[:], sqb.ap, True, True, r=[sqb.k(), "c_blk64"], w=pk(b2))
                    rs = tA[3 + (h % 2)]
                    rstd_act(rs.ap, rs.k(), ps[b2][:], pk(b2), EPS)
                    pc_ = PC_KG if is_k else PC_QG
                    gcol = pcol_sb[:, pc_:pc_ + 1]
                    if is_k:
                        S.op("dve", "scalar_tensor_tensor", kT[:, h, t0:t0 + T], ps[b][:], gcol, rs.ap,
                             ALU.mult, ALU.mult, r=pk(b) + rs.k() + ["pcol"], w=[("kT", h, I)])
                    else:
                        S.op("dve", "scalar_tensor_tensor", qn[:, h, :], ps[b][:], gcol, rs.ap,
                             ALU.mult, ALU.mult, r=pk(b) + rs.k() + ["pcol"],
                             w=qn_b.k(h * T, (h + 1) * T))

                for is_k in (False, True):
                    for h in range(4):
                        b = fm_chunk()
                        sqb = tB[(h % 2) + (2 if is_k else 0)]
                        S.op("act", "activation", sqb.ap, ps[b][:], AF.Square, r=pk(b), w=sqb.k())
                        if pend is not None:
                            qk_finish(pend)
                        pend = (b, is_k, h, sqb)
                bglr = nextbank([1, 2, 3, 4])
                for kc in range(8):
                    S.mm(ps[bglr][0:16, :], wglr_sb[:, kc * 16:(kc + 1) * 16], hn[:, kc, :], kc == 0,
                         kc == 7, r=["wglr", ("hn", kc)], w=pk(bglr))
                qk_finish(pend)
                S.op("act", "copy", glr_bf.ap[0:16, :], ps[bglr][0:16, :], r=pk(bglr), w=glr_bf.k())

                S.mm(ps[5][:], wgate_bf[:], glr_bf.ap[0:16, :], True, True, r=["wgate"] + glr_bf.k(),
                     w=pk(5))
                bufA, bufB, bufC = tA[1], tA[2], tA[0]
                S.op("act", "activation", bufA.ap, ps[5][:], AF.Exp, bias=der[:, 0:1], scale=-1.0,
                     r=pk(5) + ["der"], w=bufA.k())
                S.op("act", "activation", bufA.ap, bufA.ap, AF.Ln, bias=epsc[1.0], r=bufA.k() + ["c_eps"],
                     w=bufA.k())
                S.op("dve", "tensor_tensor_scan", bufB.ap, rmask[:], bufA.ap, 0.0, ALU.mult, ALU.add,
                     r=bufA.k() + ["c_rmask"], w=bufB.k())
                ck("gla_a")
                S.op("act", "activation", bufC.ap, bufB.ap, AF.Exp, scale=-1.0 / 16.0, r=bufB.k(),
                     w=bufC.k())
                S.op("act", "activation", bufA.ap, bufB.ap, AF.Exp, scale=1.0 / 16.0, r=bufB.k(),
                     w=bufA.k())
                S.op("dve", "scalar_tensor_tensor", qt.ap, gq_sb.ap, 32.0 ** -0.5, bufC.ap, ALU.mult,
                     ALU.mult, r=gq_sb.k() + bufC.k(), w=qt.k())
                S.op("dve", "tensor_tensor", kt.ap, gk_sb.ap, bufA.ap, ALU.mult, r=gk_sb.k() + bufA.k(),
                     w=kt.k())
                for h in range(4):
                    S.op("dve", "tensor_copy", Qblk[32 * h:32 * h + 32, :, h, :],
                         qt.ap[32 * h:32 * h + 32, :].rearrange("p (c i) -> p c i", i=128),
                         r=qt.k(), w=["Qblk"])
                ck("fm")
                for g in range(4):
                    wbuf, wkey = R_tm.next()
                    for tt in range(4):
                        b = nextbank([1, 2, 3, 4])
                        for kc in range(8):
                            S.mm(ps[b][:, 0:256], hn[:, kc, tt * 128:(tt + 1) * 128], wbuf[:, kc, :],
                                 kc == 0, kc == 7, r=[wkey, ("hn", kc)], w=pk(b, 0, 256))
                        if g == 0:
                            S.op("act", "copy", gv_sb[:, tt, :], ps[b][:, 0:256], r=pk(b, 0, 256),
                                 w=gv_b.k(tt * 256, tt * 256 + 256))
                        elif g == 1:
                            lo = (tt % 2) * 256
                            e1 = tA[5].ap[:, lo:lo + 256]
                            k1 = tA[5].k(lo, lo + 256)
                            kg = gsg_b.k(tt * 256, tt * 256 + 256)
                            S.op("act", "activation", e1, ps[b][:, 0:256], AF.Exp, scale=-1.0,
                                 r=pk(b, 0, 256), w=k1)
                            recip1p(e1, k1)
                            S.op("dve", "tensor_tensor", gsg[:, tt, :], ps[b][:, 0:256], e1, ALU.mult,
                                 r=pk(b, 0, 256) + k1, w=kg)
                            S.op("dve", "tensor_tensor", gsg[:, tt, :], gsg[:, tt, :], g8[:], ALU.mult,
                                 r=kg + ["g8"], w=kg)
                        else:
                            half = g - 2
                            eng = "act" if tt % 2 == 0 else "dve"
                            meth = "copy" if eng == "act" else "tensor_copy"
                            S.op(eng, meth, vS[:, 4 * I + tt, half * 256:(half + 1) * 256],
                                 ps[b][:, 0:256], r=pk(b, 0, 256), w=[("vS", 4 * I + tt, half)])

                ck("tm")
                ck("gla_b")
                trb = psbf(6)

                def emit_kt_transposes():
                    for c in range(4):
                        tr_op(trb[:, c * 128:(c + 1) * 128], kt.ap[:, c * 128:(c + 1) * 128],
                              kt.k() + ["c_ident"], pk(6, 0, 256))
                    S.op("act", "copy", kt_b.ap, trb[:, 0:512], r=pk(6, 0, 256), w=kt_b.k())
                def emit_diag(cc):
                    for w_ in range(31):
                        S.op("dve", "tensor_scalar", dg[:, w_, :], ident[:],
                             pcol_sb[:, PC_CW + cc * 31 + w_:PC_CW + cc * 31 + w_ + 1], None, ALU.mult,
                             r=["c_ident", "pcol"], w=dg_b.k(w_ * 128, w_ * 128 + 128))

                def emit_conv(cc):
                    by = nextbank([2, 3])
                    for w_ in range(31):
                        S.mm(ps[by][:], dg[:, w_, :], zb[:, cc, w_:w_ + T], w_ == 0, w_ == 30,
                             r=dg_b.k(w_ * 128, w_ * 128 + 128) + [("zb", cc)], w=pk(by))
                    S.op("dve", "tensor_copy", zb[:, cc, 0:30], zb[:, cc, T:T + 30], r=[("zb", cc)],
                         w=[("zb", cc)])
                    ysb = (gq_sb, gk_sb)[cc]
                    ybf = tB[2 + cc]
                    cb = pcol_sb[:, PC_CB + cc:PC_CB + cc + 1]
                    S.op("act", "activation", ysb.ap, ps[by][:], AF.Identity, bias=cb,
                         r=pk(by) + ["pcol"], w=ysb.k())
                    S.op("act", "activation", ybf.ap, ps[by][:], AF.Identity, bias=cb,
                         r=pk(by) + ["pcol"], w=ybf.k())

                    def c2():
                        bm = nextbank([2, 3])
                        S.mm(ps[bm][:], blk64[:], ybf.ap, True, True, r=ybf.k() + ["c_blk64"], w=pk(bm))
                        S.op("dve", "tensor_tensor", ysb.ap, ysb.ap, ps[bm][:], ALU.subtract,
                             r=ysb.k() + pk(bm), w=ysb.k())
                        S.op("dve", "tensor_tensor", ybf.ap, ysb.ap, ysb.ap, ALU.mult, r=ysb.k(),
                             w=ybf.k())

                        def c3():
                            bv = nextbank([2, 3])
                            S.mm(ps[bv][:], blk64[:], ybf.ap, True, True, r=ybf.k() + ["c_blk64"],
                                 w=pk(bv))
                            rs = tA[5]
                            rstd_act(rs.ap, rs.k(), ps[bv][:], pk(bv), EPS)
                            S.op("dve", "tensor_tensor", ysb.ap, ysb.ap, rs.ap, ALU.mult,
                                 r=ysb.k() + rs.k(), w=ysb.k())
                            S.op("dve", "tensor_scalar", ysb.ap, ysb.ap,
                                 pcol_sb[:, PC_CG + cc:PC_CG + cc + 1],
                                 pcol_sb[:, PC_CBETA + cc:PC_CBETA + cc + 1], ALU.mult, ALU.add,
                                 r=ysb.k() + ["pcol"], w=ysb.k())
                            S.op("act", "activation", rs.ap, ysb.ap, AF.Exp, scale=-1.0, r=ysb.k(),
                                 w=rs.k())
                            recip1p(rs.ap, rs.k())
                            S.op("dve", "tensor_tensor", mixT[:, 2 + cc, :], ysb.ap, rs.ap, ALU.mult,
                                 r=ysb.k() + rs.k(), w=mixT_b.k((2 + cc) * T, (3 + cc) * T))
                            return None
                        return c3
                    return c2

                ck("gla_c")

                def emit_gla_scores(c):
                    cs_ = slice(c * 128, (c + 1) * 128)
                    S.mm(ps[7][:], kt.ap[:, cs_], Qblk[:, c, :, :].rearrange("p h i -> p (h i)"), True, True,
                         r=kt.k() + ["Qblk"], w=pk(7))
                    scm = glr_bf
                    S.op("dve", "tensor_tensor", scm.ap.rearrange("p (h i) -> p h i", i=128),
                         ps[7][:].rearrange("p (h i) -> p h i", i=128),
                         tri[:].unsqueeze(1).broadcast_to([128, 4, 128]), ALU.mult,
                         r=pk(7) + ["c_tri"], w=scm.k())

                def emit_gla_chunk(c):
                    cs_ = slice(c * 128, (c + 1) * 128)
                    kgv = gv_b.k(c * 256, c * 256 + 256)
                    scm = glr_bf
                    og = og2[c % 2]
                    ck("gla_d")
                    for h in range(4):
                        S.mm(ps[1][32 * h:32 * h + 32, 0:64], kt_tok[:, c, 32 * h:32 * h + 32],
                             gv_sb[:, c, 64 * h:64 * h + 64], True, True,
                             r=kt_b.k() + kgv, w=pk(1), tp=(0, 32 * h))
                    ck("gla_e")
                    for h in range(4):
                        S.mm(ps[0][:, 64 * h:64 * h + 64], scm.ap[:, h * 128:(h + 1) * 128],
                             gv_sb[:, c, 64 * h:64 * h + 64], h == 0, False, r=scm.k() + kgv,
                             w=pk(0, 0, 256), skip=True)
                    S.mm(ps[0][:, 0:256], qt.ap[:, cs_], Sblk[:], False, True, r=qt.k() + ["Sblk"],
                         w=pk(0, 0, 256), skip=True)
                    ck("gla_f")
                    ebl = bufC.ap[:, c * 128 + 127:c * 128 + 128]
                    U = small.ap[:, 0:64]
                    S.op("dve", "tensor_tensor", U, ps[1][:, 0:64], S32[:], ALU.add,
                         r=pk(1) + ["S32"], w=small.k())
                    S.op("dve", "tensor_scalar", S32[:], U, ebl, None, ALU.mult, r=small.k() + bufC.k(),
                         w=["S32"])
                    for h in range(4):
                        S.op("dve", "tensor_scalar", Sblk[32 * h:32 * h + 32, 64 * h:64 * h + 64],
                             U[32 * h:32 * h + 32, :], ebl[32 * h:32 * h + 32, :], None, ALU.mult,
                             r=small.k() + bufC.k(), w=["Sblk"])
                    ck("gla_g")
                    osq_ap = mixT_b.ap[:, 7 * T:8 * T].bitcast(F32)
                    osq_k = mixT_b.k(7 * T, 8 * T)
                    osb_ap = mixT_b.ap[:, 6 * T:7 * T].bitcast(F32)
                    osb_k = mixT_b.k(6 * T, 7 * T)
                    S.op("act", "copy", osb_ap, ps[0][:, 0:256], r=pk(0, 0, 256), w=osb_k)
                    S.op("dve", "tensor_tensor", osq_ap, osb_ap, osb_ap, ALU.mult, r=osb_k, w=osq_k)
                    ss = small.ap[:, 64:68]
                    S.op("dve", "tensor_reduce", ss, osq_ap.rearrange("p (h v) -> p h v", v=64),
                         AX.X, ALU.add, r=osq_k, w=small.k())
                    rstd_act(ss, small.k(), ss, small.k(), 64.0 * EPS)
                    S.op("dve", "tensor_tensor", osq_ap.rearrange("p (h v) -> p h v", v=64),
                         osb_ap.rearrange("p (h v) -> p h v", v=64),
                         ss.unsqueeze(2).broadcast_to([128, 4, 64]), ALU.mult,
                         r=osb_k + small.k(), w=osq_k)
                    S.op("dve", "tensor_tensor", og.ap, osq_ap, gsg[:, c, :], ALU.mult,
                         r=osq_k + gsg_b.k(c * 256, c * 256 + 256), w=og.k())
                    ck("gla_h")

                    def gla_b():
                        for hh in range(2):
                            tr_op(trb[:, 512 + hh * 128:512 + (hh + 1) * 128],
                                  og.ap[:, hh * 128:(hh + 1) * 128], og.k() + ["c_ident"], pk(6, 256, 384))
                        S.op("dve", "tensor_copy", mixT[:, 0:2, cs_],
                             trb[:, 512:768].rearrange("p (a b) -> p a b", b=128),
                             r=pk(6, 256, 384), w=mixT_b.k(0, 2 * T))
                        return None
                    return gla_b

                nkb = 4 * I + 4

                def emit_head(h):
                    kqn = qn_b.k(h * T, (h + 1) * T)

                    def emit_S(j):
                        q0 = 0 if j < 4 * I else (j - 4 * I) * 128
                        for m in range(2):
                            bS = 2 * (j % 2) + m
                            S.mm(ps[bS][:, q0:T], kT[64 * m:64 * m + 64, h, j * 128:(j + 1) * 128],
                                 qn[64 * m:64 * m + 64, h, q0:T], True, True,
                                 r=[("kT", h, j // 4)] + kqn, w=pk(bS, q0, T), tp=(64 * m, 0))

                    def emit_exp(j):
                        q0 = 0 if j < 4 * I else (j - 4 * I) * 128
                        p = j % 2
                        ptb = PTp[p]
                        src = psall[:, (2 * p) * 512:(2 * p + 2) * 512].rearrange("p (m q) -> p m q", q=512)
                        dstv = ptb.ap.rearrange("p (m q) -> p m q", q=512)
                        S.op("act", "activation", dstv[:, :, q0:T], src[:, :, q0:T], AF.Exp, scale=0.125,
                             r=pk(2 * p) + pk(2 * p + 1), w=ptb.k())
                        if j >= 4 * I:
                            S.op("dve", "tensor_tensor", dstv[:, :, q0:q0 + 128], dstv[:, :, q0:q0 + 128],
                                 tri[:].unsqueeze(1).broadcast_to([128, 2, 128]), ALU.mult,
                                 r=ptb.k() + ["c_tri"], w=ptb.k())

                    def emit_AV(j):
                        diag = j >= 4 * I
                        q0 = 0 if not diag else (j - 4 * I) * 128
                        vv = vS[:, j, h * 128:(h + 1) * 128]
                        vk = ("vS", j, h // 2)
                        for m in range(2):
                            ptb = PTp[j % 2]
                            ptap = ptb.ap[:, m * T:(m + 1) * T]
                            if not diag:
                                regions = [(0, T, False)]
                            else:
                                regions = [(q0, q0 + 128, True)]
                                if q0 + 128 < T:
                                    regions.append((q0 + 128, T, False))
                            for ri, (a, b_, last) in enumerate(regions):
                                S.mm(ps[4 + m][:, a:b_], vv, ptap[:, a:b_], j == 0 and ri == 0, last,
                                     r=[vk] + ptb.k(), w=pk(4 + m, a, b_), skip=True)
                            for ri, (a, b_, last) in enumerate(regions):
                                S.mm(ps[6 + m][:, a:b_], ones1[:], ptap[:, a:b_], j == 0 and ri == 0,
                                     last, r=["c_ones1"] + ptb.k(), w=pk(6 + m, a, b_), skip=True)

                    emit_S(0)
                    for j in range(nkb):
                        if j + 1 < nkb:
                            emit_S(j + 1)
                        emit_exp(j)
                        emit_AV(j)
                    r0, r1, o0, o1 = tA[1], tA[2], tA[3 + (h % 2)], tA[5]
                    S.op("act", "activation", r0.ap, ps[6][:], AF.Ln, r=pk(6), w=r0.k())
                    S.op("dve", "tensor_copy", o0.ap, ps[4][:], r=pk(4), w=o0.k())
                    S.op("act", "activation", r1.ap, ps[7][:], AF.Ln, r=pk(7), w=r1.k())
                    S.op("dve", "tensor_copy", o1.ap, ps[5][:], r=pk(5), w=o1.k())
                    osqb = tB[h % 2]

                    def post1b():
                        return _post1b(h, r0, r1, o0, o1, osqb)
                    return post1b

                def _post1b(h, r0, r1, o0, o1, osqb):
                    S.op("act", "activation", r0.ap, r0.ap, AF.Exp, scale=-1.0, r=r0.k(), w=r0.k())
                    S.op("act", "activation", r1.ap, r1.ap, AF.Exp, scale=-1.0, r=r1.k(), w=r1.k())
                    S.op("dve", "tensor_tensor", o0.ap, o0.ap, r0.ap, ALU.mult, r=o0.k() + r0.k(),
                         w=o0.k())
                    S.op("dve", "scalar_tensor_tensor", o1.ap, o1.ap, der[:, 2:3], r1.ap, ALU.mult,
                         ALU.mult, r=o1.k() + r1.k() + ["der"], w=o1.k())
                    S.op("dve", "tensor_tensor", o0.ap, o0.ap, o1.ap, ALU.add, r=o0.k() + o1.k(),
                         w=o0.k())
                    S.op("dve", "tensor_tensor", osqb.ap, o0.ap, o0.ap, ALU.mult, r=o0.k(), w=osqb.k())

                    def head_fin():
                        bs_ = nextbank([2, 3])
                        S.mm(ps[bs_][:], ones128[:], osqb.ap, True, True, r=osqb.k() + ["c_ones128"],
                             w=pk(bs_))
                        rstd_act(r0.ap, r0.k(), ps[bs_][:], pk(bs_), EPS)
                        S.op("dve", "scalar_tensor_tensor", mixT[:, 4 + h, :], o0.ap, der[:, 1:2], r0.ap,
                             ALU.mult, ALU.mult, r=o0.k() + r0.k() + ["der"],
                             w=mixT_b.k((4 + h) * T, (5 + h) * T))
                        return None
                    return head_fin

                pending = []
                for c in range(4):
                    emit_gla_scores(c)
                    if c < 2:
                        emit_diag(c)
                    p1b = emit_head(c)
                    if c == 0:
                        emit_kt_transposes()
                    glb = emit_gla_chunk(c)
                    fin = p1b()
                    cv = emit_conv(c) if c < 2 else None
                    nxt = [f() for f in pending]
                    pending = [fin, glb] + [f for f in nxt if f is not None]
                    if cv is not None:
                        pending.append(cv)
                while pending:
                    pending = [g for g in (f() for f in pending) if g is not None]
                ck("gla")
                ck("conv")
                dbg("mix%d_%d" % (l, I), mixT, [128, 8, T], BF16, r=mixT_b.k())

                ck("attn")
                for j in range(8):
                    wbuf, wkey = R_fm.next()
                    b = nextbank([0, 1])
                    for ki, kc in enumerate((4, 5, 6, 2, 3, 7, 0, 1)):
                        S.mm(ps[b][:], wbuf[:, kc, :], mixT[:, kc, :], ki == 0, ki == 7,
                             r=[wkey] + mixT_b.k(kc * T, (kc + 1) * T), w=pk(b))
                    S.op("dve", "tensor_tensor", xc(j), xc(j), ps[b][:], ALU.add,
                         r=[xk(j)] + pk(b), w=[xk(j)])
                    norm_sq(j)
                    if j >= 2:
                        norm_mm(j - 2, 2, j == 2, False)
                norm_mm(6, 2, False, False)
                norm_mm(7, 2, False, True)

                for c_ in range(8):
                    dbg("xa%d_%d_%d" % (l, I, c_), xc(c_), [128, T], F32, r=[xk(c_)])

                ck("oproj")
                rmsnorm(PC_FFNG, 2, True, stats_done=True)
                rtv = rt.rearrange("p (a b) -> p a b", b=80)
                lg = rtv[:, 0, :].rearrange("p (t n) -> p t n", n=20)
                S.op("dve", "tensor_tensor", lg, ps[3][:, 0:80].rearrange("p (t n) -> p t n", n=20),
                     prow_sb[:, PR_RB:PR_RB + 80].rearrange("p (t n) -> p t n", n=20), ALU.add,
                     r=pk(3, 0, 128) + ["prow"], w=rt_b.k())
                RT = dict(r=rt_b.k(), w=rt_b.k())
                gl = lg[:, :, 0:4]
                gmax = rtv[:, 1, 0:4]
                goh = rtv[:, 1, 4:20].rearrange("p (t n) -> p t n", n=4)
                gd = rtv[:, 1, 20:36].rearrange("p (t n) -> p t n", n=4)
                gsum = rtv[:, 1, 36:40]
                S.op("dve", "tensor_reduce", gmax, gl, AX.X, ALU.max, **RT)
                S.op("dve", "tensor_tensor", goh, gl, gmax.unsqueeze(2).broadcast_to([128, 4, 4]),
                     ALU.is_equal, **RT)
                S.op("dve", "tensor_tensor", gd, gl, gmax.unsqueeze(2).broadcast_to([128, 4, 4]),
                     ALU.subtract, **RT)
                S.op("act", "activation", gd, gd, AF.Exp, **RT)
                S.op("dve", "tensor_reduce", gsum, gd, AX.X, ALU.add, **RT)
                S.op("dve", "reciprocal", gsum, gsum, **RT)
                el = lg[:, :, 4:20].rearrange("p t (g e) -> p t g e", e=4)
                tmp4 = rtv[:, 2, 0:64].rearrange("p (t g e) -> p t g e", g=4, e=4)
                S.op("dve", "tensor_tensor", tmp4, el, goh.unsqueeze(3).broadcast_to([128, 4, 4, 4]),
                     ALU.mult, **RT)
                sel = rtv[:, 3, 0:16].rearrange("p (t e) -> p t e", e=4)
                S.op("dve", "tensor_reduce", sel, tmp4.rearrange("p t g e -> p t e g"), AX.X, ALU.add,
                     **RT)
                m1 = rtv[:, 3, 16:20]
                mk1 = rtv[:, 3, 20:36].rearrange("p (t e) -> p t e", e=4)
                sel2 = rtv[:, 3, 36:52].rearrange("p (t e) -> p t e", e=4)
                m2 = rtv[:, 3, 52:56]
                mk2 = rtv[:, 3, 56:72].rearrange("p (t e) -> p t e", e=4)
                S.op("dve", "tensor_reduce", m1, sel, AX.X, ALU.max, **RT)
                S.op("dve", "tensor_tensor", mk1, sel, m1.unsqueeze(2).broadcast_to([128, 4, 4]),
                     ALU.is_equal, **RT)
                S.op("dve", "scalar_tensor_tensor", sel2, mk1, -1e30, sel, ALU.mult, ALU.add, **RT)
                S.op("dve", "tensor_reduce", m2, sel2, AX.X, ALU.max, **RT)
                S.op("dve", "tensor_tensor", mk2, sel2, m2.unsqueeze(2).broadcast_to([128, 4, 4]),
                     ALU.is_equal, **RT)
                dd = rtv[:, 4, 0:4]
                w1 = rtv[:, 4, 4:8]
                w2 = rtv[:, 4, 8:12]
                S.op("dve", "tensor_tensor", dd, m2, m1, ALU.subtract, **RT)
                S.op("act", "activation", dd, dd, AF.Exp, **RT)
                S.op("dve", "tensor_scalar", w1, dd, 1.0, None, ALU.add, **RT)
                S.op("dve", "reciprocal", w1, w1, **RT)
                S.op("dve", "tensor_tensor", w2, dd, w1, ALU.mult, **RT)
                S.op("dve", "tensor_tensor", w1, w1, gsum, ALU.mult, **RT)
                S.op("dve", "tensor_tensor", w2, w2, gsum, ALU.mult, **RT)
                wl = rtv[:, 4, 12:28].rearrange("p (t e) -> p t e", e=4)
                wl2 = rtv[:, 4, 28:44].rearrange("p (t e) -> p t e", e=4)
                S.op("dve", "tensor_tensor", wl, mk1, w1.unsqueeze(2).broadcast_to([128, 4, 4]),
                     ALU.mult, **RT)
                S.op("dve", "tensor_tensor", wl2, mk2, w2.unsqueeze(2).broadcast_to([128, 4, 4]),
                     ALU.mult, **RT)
                S.op("dve", "tensor_tensor", wl, wl, wl2, ALU.add, **RT)
                gates = rtv[:, 5, 0:64].rearrange("p (t g e) -> p t g e", g=4, e=4)
                S.op("dve", "tensor_tensor", gates, goh.unsqueeze(3).broadcast_to([128, 4, 4, 4]),
                     wl.unsqueeze(2).broadcast_to([128, 4, 4, 4]), ALU.mult, **RT)
                gates_te = rtv[:, 5, 0:64].rearrange("p (t n) -> p t n", n=16)
                dbg("gates%d_%d" % (l, I), rtv[:, 5, 0:64], [128, 64], F32, r=rt_b.k())

                ck("router")
                def emit_G(e):
                    De_e = De[e % 2]
                    S.op("dve", "tensor_tensor", De_e.ap.rearrange("p (c t) -> p c t", t=128),
                         ident[:].unsqueeze(1).broadcast_to([128, 4, 128]),
                         gates_te[:, :, e:e + 1].broadcast_to([128, 4, 128]), ALU.mult,
                         r=rt_b.k() + ["c_ident"], w=De_e.k())
                    bG = 4 + (e % 2)
                    for tt in range(4):
                        S.mm(ps[bG][:, tt * 128:(tt + 1) * 128], ones1[:],
                             De_e.ap[:, tt * 128:(tt + 1) * 128], True, True,
                             r=De_e.k() + ["c_ones1"], w=pk(bG, tt * 128, tt * 128 + 128))

                for e in range(NE):
                    bG = 4 + (e % 2)
                    wg_buf, wg_key = R_gu.next()
                    wu_buf, wu_key = R_gu.next()
                    for hc in range(2):
                        par = (e * 2 + hc) % 2
                        bg_, bu_ = 2 * par, 2 * par + 1
                        for kc in range(8):
                            S.mm(ps[bg_][:], wg_buf[:, hc, kc, :], hn[:, kc, :], kc == 0, kc == 7,
                                 r=[wg_key, ("hn", kc)], w=pk(bg_))
                        for kc in range(8):
                            S.mm(ps[bu_][:], wu_buf[:, hc, kc, :], hn[:, kc, :], kc == 0, kc == 7,
                                 r=[wu_key, ("hn", kc)], w=pk(bu_))
                        if hc == 0 and e == 0:
                            emit_G(0)
                        if hc == 1 and e + 1 < NE:
                            emit_G(e + 1)
                        S.op("act", "activation", sgb[par].ap, ps[bg_][:], AF.Silu, r=pk(bg_),
                             w=sgb[par].k())
                        S.op("dve", "tensor_tensor", hb[par].ap, sgb[par].ap, ps[bu_][:], ALU.mult,
                             r=sgb[par].k() + pk(bu_), w=hb[par].k())
                        eh = e * 2 + hc
                        S.op("dve", "tensor_tensor", actp[:, eh, :], hb[par].ap, ps[bG][:], ALU.mult,
                             r=hb[par].k() + pk(bG), w=actp_b.k(eh * T, (eh + 1) * T))
                for j in DOWN_ORDER:
                    by = 6 + (j % 2)
                    for half in range(2):
                        wbuf, wkey = R_wd.next()
                        for i in range(16):
                            eh = half * 16 + i
                            S.mm(ps[by][:], wbuf[:, i, :], actp[:, eh, :], eh == 0, eh == 31,
                                 r=[wkey] + actp_b.k(eh * T, (eh + 1) * T), w=pk(by))
                    if j < 6:
                        S.op("dve", "tensor_tensor", xc(j), xc(j), ps[by][:], ALU.add,
                             r=[xk(j)] + pk(by), w=[xk(j)])
                        S.dma("pool", dst[j * 128:(j + 1) * 128, t0:t0 + T], xc(j), r=[xk(j)],
                              w=[("xres", l + 1, I, j)])
                    else:
                        tmp = xn32[j - 6]
                        S.op("dve", "tensor_tensor", tmp.ap, xc(j), ps[by][:], ALU.add,
                             r=[xk(j)] + pk(by), w=tmp.k())
                        S.dma("pool", dst[j * 128:(j + 1) * 128, t0:t0 + T], tmp.ap, r=tmp.k(),
                              w=[("xres", l + 1, I, j)])
                    if I + 1 < NB:
                        nl, nI, nsrc = l, I + 1, src
                    elif l + 1 < L:
                        nl, nI, nsrc = l + 1, 0, xmid
                    else:
                        nl = None
                    if nl is not None:
                        nj = []
                        if j >= 2:
                            nj = [j - 2]
                        if j == 2:
                            nj.append(6)
                        if j == 3:
                            nj.append(7)
                        for jn in nj:
                            S.dma("pool", xc(jn, gb + 1), nsrc[jn * 128:(jn + 1) * 128, nI * T:(nI + 1) * T],
                                  r=[("xres", nl, nI, jn)] if nl > 0 else [], w=[xk(jn, gb + 1)])
                ck("moe")

        except _Stop:
            pass
        S.add("sp", None, reads=[("xres", L, I, j) for I in range(NB) for j in range(8)] +
              [("dbg", n) for n in dbg_outs] +
              [("c_wout", l_, 7) for l_ in range(L)] + [("c_wd", l_, 1) for l_ in range(L)])
        S.finalize(st)
        info = dict(ops=S.n_ops, counts=S.max_counts, arena=(mixer_top, moe_top), sbuf_left=nc.sbuf_bytes_remaining,
                    pe_tags=[o.tag for o in S.ops if o.stream == "pe" and o.fn is not None],
                    act_tags=[o.tag for o in S.ops if o.stream == "act" and o.fn is not None and not o.is_dma],
                    dve_tags=[o.tag for o in S.ops if o.stream == "dve" and o.fn is not None and not o.is_dma])
    return nc, info


def prep_weights(inp, L):
    f = np.float32
    w_in = np.asarray(inp["w_in"], f)
    out = {}

    def chunks_kp(w, cols, width):
        res = np.stack([w[:, c:c + width] for c in cols], 0)
        res = res.reshape(len(cols), 8, 128, width).transpose(0, 2, 1, 3)
        return np.ascontiguousarray(res)

    out["w_fm"] = np.stack([chunks_kp(w_in[l], FM_COLS, 128).reshape(14 * 128, 1024) for l in range(L)])
    out["w_glr"] = np.stack([chunks_kp(w_in[l], [512], 16).reshape(128, 128) for l in range(L)])
    out["w_tm"] = np.stack([chunks_kp(w_in[l], TM_COLS, 256).reshape(4 * 128, 2048) for l in range(L)])
    w_o = np.asarray(inp["w_out"], f)
    out["w_out"] = np.stack([chunks_kp(w_o[l], [128 * j for j in range(8)], 128).reshape(8 * 128, 1024)
                             for l in range(L)])
    wg = np.asarray(inp["expert_w_gate"], f)
    wu = np.asarray(inp["expert_w_up"], f)
    wdn = np.asarray(inp["expert_w_down"], f)
    gu = np.stack([wg, wu], 2)
    gu = gu.reshape(L, NE, 2, 8, 128, 2, 128)
    gu = gu.transpose(0, 1, 2, 4, 5, 3, 6)
    out["w_gu"] = np.ascontiguousarray(gu).reshape(L, 32 * 128, 2048)
    wd = wdn.reshape(L, 2, 8, 2, 128, 8, 128)
    wd = wd.transpose(0, 5, 1, 4, 2, 3, 6)
    out["w_d"] = np.ascontiguousarray(wd).reshape(L, 16 * 128, 2048)
    wr = np.concatenate([np.asarray(inp["router_group_w"], f), np.asarray(inp["router_expert_w"], f)], -1)
    wr = wr.reshape(L, 8, 128, 20).transpose(0, 2, 1, 3)
    out["w_r"] = np.ascontiguousarray(wr).reshape(L, 128, 160)
    out["w_gate"] = np.ascontiguousarray(np.asarray(inp["gla_gate_w"], f))
    pc = np.zeros((L, 128, NPC), f)
    pr = np.zeros((L, 128, NPR), f)
    for l in range(L):
        pc[l, :, PC_MIXG:PC_MIXG + 8] = np.asarray(inp["mix_norm_g"][l], f).reshape(8, 128).T
        pc[l, :, PC_FFNG:PC_FFNG + 8] = np.asarray(inp["ffn_norm_g"][l], f).reshape(8, 128).T
        pc[l, :, PC_GB] = np.asarray(inp["gla_gate_b"][l], f)
        pc[l, :, PC_CB:PC_CB + 2] = np.asarray(inp["conv_b"][l], f).reshape(2, 128).T
        pc[l, :, PC_CG:PC_CG + 2] = np.asarray(inp["conv_norm_g"][l], f).reshape(2, 128).T
        pc[l, :, PC_CBETA:PC_CBETA + 2] = np.asarray(inp["conv_norm_b"][l], f).reshape(2, 128).T
        pc[l, :, PC_QG] = np.tile(np.asarray(inp["diff_qnorm_g"][l], f), 2)
        pc[l, :, PC_KG] = np.tile(np.asarray(inp["diff_knorm_g"][l], f), 2)
        pc[l, :, PC_SUB] = np.asarray(inp["diff_subln_g"][l], f)
        cw = np.asarray(inp["conv_w"][l], f)
        pc[l, :, PC_CW:PC_CW + 62] = cw.reshape(31, 2, 128).transpose(2, 1, 0).reshape(128, 62)
        pr[l, :, PR_G8:PR_G8 + 256] = np.tile(np.asarray(inp["gla_norm_g"][l], f), 4)[None, :]
        rb = np.concatenate([np.asarray(inp["router_group_b"][l], f), np.asarray(inp["router_expert_b"][l], f)])
        pr[l, :, PR_RB:PR_RB + 80] = np.tile(rb, 4)[None, :]
        lq = np.concatenate([np.asarray(inp[k][l], f) for k in ("diff_lq1", "diff_lk1", "diff_lq2", "diff_lk2")])
        pr[l, :, PR_LQ:PR_LQ + 256] = lq[None, :]
    out["pcol"] = pc
    out["prow"] = pr
    return out


_CACHE = {}


def run(inputs, SEQ, DEPTH, n_cores, debug=(), trace=False, stop=None):
    key = (SEQ, DEPTH, tuple(debug), stop)
    if key not in _CACHE:
        _CACHE[key] = build(SEQ, DEPTH, debug, stop)
    nc, info = _CACHE[key]
    wts = prep_weights(inputs, DEPTH)
    x = np.asarray(inputs["x"], np.float32)
    in_maps = []
    for c in range(n_cores):
        m = dict(wts)
        m["xT"] = np.ascontiguousarray(x[c].T)
        in_maps.append(m)
    res = run_bass_kernel_spmd(nc, in_maps, core_ids=list(range(n_cores)), trace=trace)
    y = np.stack([np.asarray(r["yT"]).T for r in res.results], 0)
    return np.ascontiguousarray(y.astype(np.float32)), res, info


def kernel(**inputs):
    x = np.asarray(inputs["x"])
    B, SEQ, _ = x.shape
    DEPTH = np.asarray(inputs["w_in"]).shape[0]
    y, _, _ = run(inputs, SEQ, DEPTH, B)
    return y
```

```python
import contextlib
import math
import numpy as np
import concourse.bass as bass
import concourse.mybir as mybir
from concourse.bass_utils import run_bass_kernel_spmd

F32 = mybir.dt.float32
BF16 = mybir.dt.bfloat16
ALU = mybir.AluOpType
AF = mybir.ActivationFunctionType
AX = mybir.AxisListType

COMPUTE = ("pe", "act", "dve", "pool")
STREAMS = ("pe", "act", "dve", "pool", "sp")


class _Op:
    __slots__ = ("stream", "fn", "reads", "writes", "is_dma", "idx", "sidx", "signal",
                 "deps", "count", "dsem", "dval", "dprev", "tag")

    def __init__(self, stream, fn, reads, writes, is_dma):
        self.stream = stream
        self.fn = fn
        self.reads = reads
        self.writes = writes
        self.is_dma = is_dma
        self.signal = is_dma
        self.deps = []
        self.count = None
        self.dsem = None
        self.dval = None
        self.dprev = None


def _flat(keys):
    out = []
    for k in keys:
        if isinstance(k, list):
            out.extend(_flat(k))
        else:
            out.append(k)
    return out


class Sched:
    def __init__(self, nc, n_dma_sems=8):
        self.nc = nc
        self.ops = []
        self.n_dma_sems = n_dma_sems
        self.tag = "init"
        self.eng = {"pe": nc.tensor, "act": nc.scalar, "dve": nc.vector, "pool": nc.gpsimd,
                    "sp": nc.sync}

    def add(self, stream, fn, reads=(), writes=(), dma=False):
        op = _Op(stream, fn, tuple(_flat(reads)), tuple(_flat(writes)), dma)
        op.tag = self.tag
        op.idx = len(self.ops)
        self.ops.append(op)
        return op

    def op(self, stream, method, *args, r=(), w=(), **kw):
        return self.add(stream, lambda e: getattr(e, method)(*args, **kw), r, w)

    def dma(self, stream, out, in_, r=(), w=(), **kw):
        return self.add(stream, lambda e: e.dma_start(out=out, in_=in_, **kw), r, w, dma=True)

    def mm(self, out, lhsT, rhs, start, stop, r=(), w=(), tp=None, skip=False):
        kw = {}
        if tp is not None:
            kw["tile_position"] = tp
        if skip:
            kw["skip_group_check"] = True
        fn = lambda e: e.matmul(out, lhsT, rhs, start=start, stop=stop, **kw)
        op = self.add("pe", fn, r, w)
        op.tag = (op.tag, "%d*%d*%d" % (lhsT.shape[0], lhsT.shape[1], rhs.shape[-1] if len(rhs.shape) == 2
                                          else int(np.prod(rhs.shape[1:]))))
        return op

    def finalize(self, stack):
        nc = self.nc
        ops = self.ops
        last_w = {}
        readers = {}
        for op in ops:
            deps = set()
            for k in op.reads:
                w = last_w.get(k)
                if w is not None:
                    deps.add((w, "raw"))
            for k in op.writes:
                w = last_w.get(k)
                if w is not None:
                    deps.add((w, "waw"))
                for r in readers.get(k, ()):
                    deps.add((r, "war"))
            for k in op.reads:
                readers.setdefault(k, []).append(op.idx)
            for k in op.writes:
                last_w[k] = op.idx
                readers[k] = []
            best = {}
            dma_deps = set()
            for (d, kind) in deps:
                if d == op.idx:
                    continue
                p = ops[d]
                if p.is_dma:
                    dma_deps.add(d)
                    continue
                if p.stream == op.stream and not op.is_dma:
                    if p.stream == "pe":
                        continue
                    if kind != "raw":
                        continue
                if best.get(p.stream, -1) < d:
                    best[p.stream] = d
            op.deps = sorted(best.values()) + sorted(dma_deps)
        known = {s: {} for s in STREAMS}
        cnt = {s: 0 for s in STREAMS}
        for op in ops:
            cnt[op.stream] += 1
            op.sidx = cnt[op.stream]
        for op in ops:
            kept = []
            kn = known[op.stream]
            for d in op.deps:
                p = ops[d]
                if p.is_dma:
                    key = ("dma", d)
                    if key in kn:
                        continue
                    kn[key] = True
                    kept.append(d)
                else:
                    if kn.get(p.stream, 0) >= p.sidx:
                        continue
                    kn[p.stream] = p.sidx
                    kept.append(d)
                    p.signal = True
            op.deps = kept
        sems = {}
        for s in COMPUTE:
            sems[s] = stack.enter_context(nc.semaphore("sem_" + s))
        c = {s: 0 for s in COMPUTE}
        dma_pool = {}
        dma_state = {}
        for op in ops:
            if op.is_dma:
                pool = dma_pool.get(op.stream)
                if pool is None:
                    pool = [stack.enter_context(nc.semaphore("dsem_%s_%d" % (op.stream, i)))
                            for i in range(self.n_dma_sems)]
                    dma_pool[op.stream] = pool
                    dma_state[op.stream] = [0, [0] * self.n_dma_sems]
                st = dma_state[op.stream]
                i = st[0] % self.n_dma_sems
                st[0] += 1
                op.dsem = pool[i]
                op.dprev = st[1][i]
                st[1][i] += 16
                op.dval = st[1][i]
            elif op.signal:
                c[op.stream] += 1
                op.count = c[op.stream]
        self.max_counts = dict(c)
        self.n_ops = dict(cnt)
        assert max(c.values()) < 60000, c
        for op in ops:
            e = self.eng[op.stream]
            for d in op.deps:
                p = ops[d]
                if p.is_dma:
                    e.wait_ge(p.dsem, p.dval)
                else:
                    e.wait_ge(sems[p.stream], p.count)
            if op.is_dma:
                if op.dprev > 0:
                    e.wait_ge(op.dsem, op.dprev)
                ins = op.fn(e)
                ins.then_inc(op.dsem, 16)
            else:
                if op.fn is None:
                    continue
                ins = op.fn(e)
                if op.signal:
                    ins.then_inc(sems[op.stream], 1)
        return self


D = 1024
T = 512
NIN = 2832
EPS = 1e-6
NE = 16
FM_COLS = [0, 128, 784, 912, 1040, 1168] + [1296 + 128 * h for h in range(4)] + \
          [1808 + 128 * h for h in range(4)]
FM_GQ, FM_GK, FM_A0, FM_A1, FM_G0, FM_G1, FM_DQ, FM_DK = 0, 1, 2, 3, 4, 5, 6, 10
TM_COLS = [256, 528, 2320, 2576]
PC_MIXG, PC_FFNG, PC_GB, PC_CB, PC_CG, PC_CBETA, PC_QG, PC_KG, PC_SUB, PC_CW = 0, 8, 16, 17, 19, 21, 23, 24, 25, 26
NPC = 26 + 62
PR_G8, PR_RB, PR_LQ = 0, 256, 336
NPR = 336 + 256


def lambda_init(l):
    return 0.8 - 0.6 * math.exp(-0.3 * l)


class _Stop(Exception):
    pass


def build(SEQ, DEPTH, debug=(), stop=None):
    NB = SEQ // T
    NT = SEQ // 128
    nc = bass.Bass("TRN2", target_bir_lowering=False)
    L = DEPTH

    def din(name, shape, dt=F32):
        return nc.dram_tensor(name, shape, dt, kind="ExternalInput").ap()

    def dint(name, shape, dt):
        return nc.dram_tensor(name, shape, dt, kind="Internal").ap()

    xT_d = din("xT", [D, SEQ])
    w_fm = din("w_fm", [L, 14 * 128, 1024])
    w_glr = din("w_glr", [L, 128, 128])
    w_tm = din("w_tm", [L, 4 * 128, 2048])
    w_out = din("w_out", [L, 8 * 128, 1024])
    w_gu = din("w_gu", [L, 32 * 128, 2048])
    w_d = din("w_d", [L, 16 * 128, 2048])
    w_r = din("w_r", [L, 128, 160])
    w_gate = din("w_gate", [L, 16, 128])
    pcol = din("pcol", [L, 128, NPC])
    prow = din("prow", [L, 128, NPR])
    w_fm_b = dint("w_fm_b", [L, 14 * 128, 1024], BF16)
    w_glr_b = dint("w_glr_b", [L, 128, 128], BF16)
    w_tm_b = dint("w_tm_b", [L, 4 * 128, 2048], BF16)
    w_out_b = dint("w_out_b", [L, 8 * 128, 1024], BF16)
    w_gu_b = dint("w_gu_b", [L, 32 * 128, 2048], BF16)
    w_d_b = dint("w_d_b", [L, 16 * 128, 2048], BF16)
    xmid = dint("xmid", [D, SEQ], F32)
    yT_d = nc.dram_tensor("yT", [D, SEQ], F32, kind="ExternalOutput").ap()
    dbg_outs = {}

    st = contextlib.ExitStack()
    with st:
        S = Sched(nc)

        def sb(name, shape, dt):
            return st.enter_context(nc.sbuf_tensor(name, shape, dt))

        psall = st.enter_context(nc.psum_tensor("psall", [128, 4096], F32))

        class _Bank:
            def __init__(self, b):
                self.b = b

            def __getitem__(self, key):
                return psall[:, self.b * 512:(self.b + 1) * 512][key]

        ps = [_Bank(b) for b in range(8)]

        def pk(b, lo=0, hi=512):
            return [("ps", b)]

        def psbf(b):
            return psall[:, b * 512:(b + 1) * 512].bitcast(BF16)

        kT = sb("kT", [128, 4, SEQ], BF16)
        vS = sb("vS", [128, NT, 512], BF16)
        NSLOT = 10
        xT = sb("xT_sb", [128, NSLOT, T], F32)
        hn = sb("hn", [128, 8, T], BF16)
        ident = sb("ident", [128, 128], BF16)
        tri = sb("tri", [128, 128], BF16)
        ones1 = sb("ones1", [128, 128], BF16)
        onesD = sb("onesD", [128, 128], BF16)
        ones128 = sb("ones128", [128, 128], BF16)
        blk64 = sb("blk64", [128, 128], BF16)
        rmask = sb("rmask", [128, T], F32)
        cst = sb("cst", [128, 4], F32)
        pcol_sb = sb("pcol_sb", [128, NPC], F32)
        prow_sb = sb("prow_sb", [128, NPR], F32)
        wr_sb = sb("wr_sb", [128, 160], F32)
        wgate_bf = sb("wgate_bf", [16, 128], BF16)
        wgate_f = sb("wgate_f", [16, 128], F32)
        wglr_sb = sb("wglr_sb", [128, 128], BF16)
        der = sb("der", [128, 8], F32)
        g8 = sb("g8", [128, 256], F32)
        S32 = sb("S32", [128, 64], F32)
        Sblk = sb("Sblk", [128, 256], BF16)
        Qblk = sb("Qblk", [128, 4, 4, 128], BF16)
        zb = sb("zb", [128, 2, 30 + T], BF16)
        lamt = sb("lamt", [128, 128], F32)
        NFM, NTM, NGU, NWD = 4, 2, 4, 3
        wfm_r = [sb("wfm_r%d" % i, [128, 8, 128], BF16) for i in range(NFM)]
        wtm_r = [sb("wtm_r%d" % i, [128, 8, 256], BF16) for i in range(NTM)]
        wgu_r = [sb("wgu_r%d" % i, [128, 2, 8, 128], BF16) for i in range(NGU)]
        wd_r = [sb("wd_r%d" % i, [128, 16, 128], BF16) for i in range(NWD)]
        ARENA = 14080 + 128
        arena = sb("arena", [128, ARENA], F32)

        GR = 128

        class Buf:
            def __init__(self, ap, base, dt, cols):
                self.ap, self.base, self.dt, self.cols = ap, base, dt, cols
                self.per = 2 if dt == BF16 else 1

            def k(self, lo=0, hi=None):
                hi = self.cols if hi is None else hi
                a = self.base + lo // self.per
                b = self.base + (hi + self.per - 1) // self.per
                return [("ar", g) for g in range(a // GR, (b + GR - 1) // GR)]

        class Arena:
            def __init__(self):
                self.off = 0

            def _take(self, c32):
                c32 = ((c32 + GR - 1) // GR) * GR
                a = self.off
                self.off += c32
                assert self.off <= ARENA, self.off
                return a, c32

            def f32(self, cols):
                a, c32 = self._take(cols)
                return Buf(arena[:, a:a + cols], a, F32, cols)

            def bf16(self, cols):
                a, c32 = self._take((cols + 1) // 2)
                return Buf(arena[:, a:a + (cols + 1) // 2].bitcast(BF16), a, BF16, cols)

        A = Arena()
        mixT_b = A.bf16(8 * T)
        mixT = mixT_b.ap.rearrange("p (c t) -> p c t", t=T)
        gq_sb = A.f32(T)
        gk_sb = A.f32(T)
        glr_bf = A.bf16(T)
        gv_b = A.bf16(4 * 256)
        gv_sb = gv_b.ap.rearrange("p (c t) -> p c t", t=256)
        gsg_b = A.f32(4 * 256)
        gsg = gsg_b.ap.rearrange("p (c t) -> p c t", t=256)
        qn_b = A.bf16(4 * T)
        qn = qn_b.ap.rearrange("p (c t) -> p c t", t=T)
        tA = [A.f32(T) for _ in range(6)]
        tB = [A.bf16(T) for _ in range(4)]
        PTp = [A.bf16(2 * T) for _ in range(2)]
        qt = A.bf16(T)
        kt = A.bf16(T)
        kt_b = A.bf16(4 * 128)
        kt_tok = kt_b.ap.rearrange("p (c t) -> p c t", t=128)
        og2 = [A.bf16(256) for _ in range(2)]
        small = A.f32(128)
        dg_b = A.bf16(31 * 128)
        dg = dg_b.ap.rearrange("p (c t) -> p c t", t=128)
        mixer_top = A.off
        M = Arena()
        actp_b = M.bf16(32 * T)
        actp = actp_b.ap.rearrange("p (c t) -> p c t", t=T)
        xn32 = [M.f32(T) for _ in range(2)]
        sgb = [M.f32(T) for _ in range(2)]
        hb = [M.f32(T) for _ in range(2)]
        De = [M.bf16(4 * 128) for _ in range(2)]
        rt_b = M.f32(640)
        rt = rt_b.ap
        moe_top = M.off

        def dbg(name, ap, shape, dt=F32, r=()):
            if name not in debug:
                return
            t = nc.dram_tensor("dbg_" + name, list(shape), dt, kind="ExternalOutput").ap()
            dbg_outs[name] = t
            S.dma("sp", t, ap, r=list(r), w=[("dbg", name)])

        S.op("pool", "memset", ones1[:], 1.0, w=["c_ones1"])
        S.op("pool", "memset", onesD[:], 1.0 / 1024.0, w=["c_onesD"])
        S.op("pool", "memset", ones128[:], 1.0 / 128.0, w=["c_ones128"])
        S.op("pool", "memset", ident[:], 1.0, w=["c_ident"])
        S.op("pool", "affine_select", ident[:], ident[:], [[-1, 128]], ALU.is_equal, 0.0, base=0,
             channel_multiplier=1, r=["c_ident"], w=["c_ident"])
        S.op("pool", "memset", tri[:], 1.0, w=["c_tri"])
        S.op("pool", "affine_select", tri[:], tri[:], [[1, 128]], ALU.is_ge, 0.0, base=0,
             channel_multiplier=-1, r=["c_tri"], w=["c_tri"])
        S.op("pool", "memset", blk64[:], 0.0, w=["c_blk64"])
        S.op("pool", "memset", blk64[0:64, 0:64], 1.0 / 64.0, r=["c_blk64"], w=["c_blk64"])
        S.op("pool", "memset", blk64[64:128, 64:128], 1.0 / 64.0, r=["c_blk64"], w=["c_blk64"])
        S.op("pool", "memset", Qblk[:].rearrange("p a b c -> p (a b c)"), 0.0, w=["Qblk"])
        S.op("pool", "memset", rmask[:], 1.0, w=["c_rmask"])
        S.op("pool", "memset", rmask[:].rearrange("p (c t) -> p c t", t=128)[:, :, 0:1], 0.0,
             r=["c_rmask"], w=["c_rmask"])

        S.op("pool", "memset", cst[:, 0:1], EPS, w=["c_eps"])
        S.op("pool", "memset", cst[:, 1:2], 64.0 * EPS, r=["c_eps"], w=["c_eps"])
        S.op("pool", "memset", cst[:, 2:3], 1.0, r=["c_eps"], w=["c_eps"])
        S.op("pool", "memset", cst[:, 3:4], 0.0, r=["c_eps"], w=["c_eps"])
        epsc = {EPS: cst[:, 0:1], 64.0 * EPS: cst[:, 1:2], 1.0: cst[:, 2:3]}

        def flat128(ap):
            return ap.rearrange("(p a) c -> p (a c)", p=128)

        for c_ in range(8):
            S.dma("sp", xT[:, c_, :], xT_d[c_ * 128:(c_ + 1) * 128, 0:T], w=[("xT", c_)])

        def cast_parts(l):
            def proj():
                for j in range(14):
                    S.dma("pool", w_fm_b[l, j * 128:(j + 1) * 128, :], w_fm[l, j * 128:(j + 1) * 128, :],
                          r=[("xT", c_) for c_ in range(8)] if (l == 0 and j == 2) else [],
                          w=[("c_wfm", l, j)])
                    if j == 0:
                        S.dma("pool", w_glr_b[l], w_glr[l], w=[("c_wglr", l)])
                for j in range(4):
                    S.dma("pool", w_tm_b[l, j * 128:(j + 1) * 128, :], w_tm[l, j * 128:(j + 1) * 128, :],
                          w=[("c_wtm", l, j)])
                for j in range(8):
                    S.dma("pool", w_out_b[l, j * 128:(j + 1) * 128, :], w_out[l, j * 128:(j + 1) * 128, :],
                          w=[("c_wout", l, j)])

            def gu(g):
                def f():
                    S.dma("pool", flat128(w_gu_b[l, g * 1024:(g + 1) * 1024]),
                          flat128(w_gu[l, g * 1024:(g + 1) * 1024]),
                          r=[("c_wfm", l, 13), ("c_wtm", l, 3), ("c_wout", l, 7)] if g == 0
                          else [("c_wgu", l, g - 1)],
                          w=[("c_wgu", l, g)])
                return f

            def wd(g):
                def f():
                    S.dma("pool", flat128(w_d_b[l, g * 1024:(g + 1) * 1024]),
                          flat128(w_d[l, g * 1024:(g + 1) * 1024]),
                          r=[("c_wgu", l, 1)] if g == 0 else [("c_wd", l, 0), ("c_wgu", l, 3)],
                          w=[("c_wd", l, g)])
                return f
            return [proj, gu(0), gu(1), gu(2), gu(3), wd(0), wd(1)]

        def emit_casts(l):
            for f in cast_parts(l):
                f()

        emit_casts(0)

        class Ring:
            def __init__(self, name, bufs, items, loader, hold=1):
                self.name, self.bufs, self.items, self.loader = name, bufs, items, loader
                self.hold = hold
                self.n_loaded = 0
                self.n_used = 0

            def key(self, i):
                return (self.name, i % len(self.bufs))

            def prefetch(self, upto):
                upto = min(upto, len(self.items))
                while self.n_loaded < upto:
                    i = self.n_loaded
                    self.loader(self.bufs[i % len(self.bufs)], self.items[i], self.key(i))
                    self.n_loaded += 1

            def next(self):
                i = self.n_used
                self.prefetch(i + len(self.bufs) - (self.hold - 1))
                self.n_used += 1
                return self.bufs[i % len(self.bufs)], self.key(i)

        DOWN_ORDER = (2, 3, 4, 5, 6, 7, 0, 1)
        fm_items = []
        tm_items = []
        gu_items = []
        wd_items = []
        for l in range(L):
            for I in range(NB):
                for j in range(14):
                    fm_items.append(("fm", l, j))
                for j in range(8):
                    fm_items.append(("out", l, j))
                for g in range(4):
                    tm_items.append((l, g))
                for e in range(NE):
                    for gu in range(2):
                        gu_items.append((l, e, gu))
                for j in DOWN_ORDER:
                    for half in range(2):
                        wd_items.append((l, j, half))

        def load_fm(buf, it, key):
            kind, l, j = it
            if kind == "fm":
                S.dma("sp", buf[:].rearrange("p a b -> p (a b)"), w_fm_b[l, j * 128:(j + 1) * 128, :],
                      r=[("c_wfm", l, j)], w=[key])
            else:
                S.dma("sp", buf[:].rearrange("p a b -> p (a b)"), w_out_b[l, j * 128:(j + 1) * 128, :],
                      r=[("c_wout", l, j)], w=[key])

        def load_tm(buf, it, key):
            l, g = it
            S.dma("sp", buf[:].rearrange("p a b -> p (a b)"), w_tm_b[l, g * 128:(g + 1) * 128, :],
                  r=[("c_wtm", l, g)], w=[key])

        def load_gu(buf, it, key):
            l, e, gu = it
            row = (e * 2 + gu) * 128
            S.dma("sp", buf[:].rearrange("p a b c -> p (a b c)"), w_gu_b[l, row:row + 128, :],
                  r=[("c_wgu", l, e // 4)], w=[key])

        def load_wd(buf, it, key):
            l, j, half = it
            row = (j * 2 + half) * 128
            S.dma("sp", buf[:].rearrange("p a b -> p (a b)"), w_d_b[l, row:row + 128, :],
                  r=[("c_wd", l, j // 4)], w=[key])

        R_fm = Ring("r_fm", wfm_r, fm_items, load_fm)
        R_tm = Ring("r_tm", wtm_r, tm_items, load_tm)
        R_gu = Ring("r_gu", wgu_r, gu_items, load_gu, hold=2)
        R_wd = Ring("r_wd", wd_r, wd_items, load_wd)

        bank_rr = [0]

        def nextbank(choices):
            b = choices[bank_rr[0] % len(choices)]
            bank_rr[0] += 1
            return b

        ev = [0]

        def alt(engs=("dve", "pool")):
            ev[0] += 1
            return engs[ev[0] % len(engs)]

        def rstd_act(dst, dkeys, src, skeys, eps):
            S.op("act", "activation", dst, src, AF.Ln, bias=epsc[eps], r=skeys + ["c_eps"], w=dkeys)
            S.op("act", "activation", dst, dst, AF.Exp, scale=-0.5, r=dkeys, w=dkeys)

        def recip1p(dst, dkeys):
            S.op("act", "activation", dst, dst, AF.Ln, bias=epsc[1.0], r=dkeys + ["c_eps"], w=dkeys)
            S.op("act", "activation", dst, dst, AF.Exp, scale=-1.0, r=dkeys, w=dkeys)

        def ck(name):
            S.tag = "post_" + name
            if stop == name:
                raise _Stop()

        def tr_op(o, i_, r, w):
            S.add("pe", lambda e: e.transpose(o, i_, ident[:]), r, w)

        try:
          ck("casts")
          for l in range(L):
            src = xT_d if l == 0 else xmid
            dst = yT_d if l == L - 1 else xmid
            li = lambda_init(l)
            S.dma("sp", pcol_sb[:], pcol[l], w=["pcol"])
            S.dma("sp", prow_sb[:], prow[l], w=["prow"])
            S.dma("sp", wr_sb[:], w_r[l], w=["wr"])
            S.dma("sp", wgate_f[:], w_gate[l], w=["wgate_f"])
            S.op("dve", "tensor_copy", wgate_bf[:], wgate_f[:], r=["wgate_f"], w=["wgate"])
            S.dma("sp", wglr_sb[:], w_glr_b[l], r=[("c_wglr", l)], w=["wglr"])
            S.op("dve", "tensor_scalar", der[:, 0:1], pcol_sb[:, PC_GB:PC_GB + 1], -1.0, None, ALU.mult,
                 r=["pcol"], w=["der"])
            S.op("dve", "tensor_scalar", der[:, 1:2], pcol_sb[:, PC_SUB:PC_SUB + 1], 1.0 - li, None,
                 ALU.mult, r=["pcol"], w=["der"])
            S.op("dve", "tensor_scalar", g8[:], prow_sb[:, PR_G8:PR_G8 + 256], 8.0, None, ALU.mult,
                 r=["prow"], w=["g8"])
            S.op("dve", "tensor_tensor", lamt[:, 0:64], prow_sb[:, PR_LQ:PR_LQ + 64],
                 prow_sb[:, PR_LQ + 64:PR_LQ + 128], ALU.mult, r=["prow"], w=["lamt"])
            S.op("dve", "tensor_tensor", lamt[:, 64:128], prow_sb[:, PR_LQ + 128:PR_LQ + 192],
                 prow_sb[:, PR_LQ + 192:PR_LQ + 256], ALU.mult, r=["prow", "lamt"], w=["lamt"])
            S.op("dve", "tensor_reduce", der[:, 4:6], lamt[:].rearrange("p (a b) -> p a b", b=64),
                 AX.X, ALU.add, r=["lamt"], w=["der"])
            S.op("act", "activation", der[:, 6:8], der[:, 4:6], AF.Exp, r=["der"], w=["der"])
            S.op("dve", "tensor_tensor", der[:, 2:3], der[:, 7:8], der[:, 6:7], ALU.subtract,
                 r=["der"], w=["der"])
            S.op("dve", "tensor_scalar", der[:, 2:3], der[:, 2:3], -li, None, ALU.add,
                 r=["der"], w=["der"])
            S.op("dve", "memset", S32[:], 0.0, w=["S32"])
            S.op("dve", "memset", Sblk[:], 0.0, w=["Sblk"])
            S.op("dve", "memset", zb[:, :, 0:30], 0.0, w=[("zb", 0), ("zb", 1)])

            ck("params")
            for I in range(NB):
                t0 = I * T
                gb = l * NB + I

                def xslot(j, g=None):
                    return (j + 2 * (gb if g is None else g)) % NSLOT

                def xc(j, g=None):
                    return xT[:, xslot(j, g), :]

                def xk(j, g=None):
                    return ("xT", xslot(j, g))

                if l + 1 < L:
                    if NB >= 8:
                        if 1 <= I <= 7:
                            cast_parts(l + 1)[I - 1]()
                    elif I == min(1, NB - 1):
                        emit_casts(l + 1)

                def norm_sq(kc):
                    S.op("act", "activation", hn[:, kc, :], xc(kc), AF.Square,
                         r=[xk(kc)], w=[("hn", kc)])

                def norm_mm(kc, bank, first, last):
                    S.mm(ps[bank][:], onesD[:], hn[:, kc, :], first, last,
                         r=[("hn", kc), "c_onesD"], w=pk(bank))

                def rmsnorm(gbase, bank, router, stats_done=False):
                    order = (6, 7, 0, 1, 2, 3, 4, 5)
                    if not stats_done:
                        for kc in order:
                            S.op("act", "activation", hn[:, kc, :], xc(kc), AF.Square,
                                 r=[xk(kc)], w=[("hn", kc)])
                        for ki, kc in enumerate(order):
                            S.mm(ps[bank][:], onesD[:], hn[:, kc, :], ki == 0, ki == 7,
                                 r=[("hn", kc), "c_onesD"], w=pk(bank))
                    rstd = tA[0]
                    rstd_act(rstd.ap, rstd.k(), ps[bank][:], pk(bank), EPS)
                    for kc in range(8):
                        gc = pcol_sb[:, gbase + kc:gbase + kc + 1]
                        if not router:
                            S.op("dve", "scalar_tensor_tensor", hn[:, kc, :], xc(kc), gc, rstd.ap,
                                 ALU.mult, ALU.mult, r=[xk(kc), rstd.k(), "pcol"], w=[("hn", kc)])
                        else:
                            xb = xn32[kc % 2]
                            S.op("dve", "scalar_tensor_tensor", xb.ap, xc(kc), gc, rstd.ap,
                                 ALU.mult, ALU.mult, r=[xk(kc), rstd.k(), "pcol"], w=xb.k())
                            S.op("act", "copy", hn[:, kc, :], xb.ap, r=xb.k(), w=[("hn", kc)])
                            for tt in range(4):
                                S.mm(ps[3][:, tt * 20:(tt + 1) * 20], xb.ap[:, tt * 128:(tt + 1) * 128],
                                     wr_sb[:, kc * 20:(kc + 1) * 20], kc == 0 and tt == 0,
                                     kc == 7 and tt == 3, r=[xb.k(), "wr"], w=pk(3, 0, 128), skip=True)

                ck("load")
                rmsnorm(PC_MIXG, 0, False)
                ck("norm1")
                dbg("hn%d_%d" % (l, I), hn[:], [128, 8, T], BF16, r=[("hn", c) for c in range(8)])

                def fm_chunk():
                    wbuf, wkey = R_fm.next()
                    b = nextbank([0, 1, 2, 3, 4, 7])
                    for kc in range(8):
                        S.mm(ps[b][:], wbuf[:, kc, :], hn[:, kc, :], kc == 0, kc == 7,
                             r=[wkey, ("hn", kc)], w=pk(b))
                    return b

                b = fm_chunk()
                S.op("act", "copy", gq_sb.ap, ps[b][:], r=pk(b), w=gq_sb.k())
                b = fm_chunk()
                S.op("act", "copy", gk_sb.ap, ps[b][:], r=pk(b), w=gk_sb.k())
                ba = [fm_chunk(), fm_chunk()]
                for cc in range(2):
                    bg = fm_chunk()
                    tt_ = tA[1 + cc]
                    S.op("act", "activation", tt_.ap, ps[bg][:], AF.Exp, scale=-1.0, r=pk(bg), w=tt_.k())
                    recip1p(tt_.ap, tt_.k())
                    S.op("dve", "tensor_tensor", zb[:, cc, 30:30 + T], ps[ba[cc]][:], tt_.ap,
                         ALU.mult, r=pk(ba[cc]) + tt_.k(), w=[("zb", cc)])
                pend = None

                def qk_finish(p):
                    b, is_k, h, sqb = p
                    b2 = nextbank([5, 6])
                    S.mm(ps[b2][:], blk64[:], sqb.ap, True, True, r=[sqb.k(), "c_blk64"], w=pk(b2))
                    rs = tA[3 + (h % 2)]
                    rstd_act(rs.ap, rs.k(), ps[b2][:], pk(b2), EPS)
                    pc_ = PC_KG if is_k else PC_QG
                    gcol = pcol_sb[:, pc_:pc_ + 1]
                    if is_k:
                        S.op("dve", "scalar_tensor_tensor", kT[:, h, t0:t0 + T], ps[b][:], gcol, rs.ap,
                             ALU.mult, ALU.mult, r=pk(b) + rs.k() + ["pcol"], w=[("kT", h, I)])
                    else:
                        S.op("dve", "scalar_tensor_tensor", qn[:, h, :], ps[b][:], gcol, rs.ap,
                             ALU.mult, ALU.mult, r=pk(b) + rs.k() + ["pcol"],
                             w=qn_b.k(h * T, (h + 1) * T))

                for is_k in (False, True):
                    for h in range(4):
                        b = fm_chunk()
                        sqb = tB[(h % 2) + (2 if is_k else 0)]
                        S.op("act", "activation", sqb.ap, ps[b][:], AF.Square, r=pk(b), w=sqb.k())
                        if pend is not None:
                            qk_finish(pend)
                        pend = (b, is_k, h, sqb)
                bglr = nextbank([1, 2, 3, 4])
                for kc in range(8):
                    S.mm(ps[bglr][0:16, :], wglr_sb[:, kc * 16:(kc + 1) * 16], hn[:, kc, :], kc == 0,
                         kc == 7, r=["wglr", ("hn", kc)], w=pk(bglr))
                qk_finish(pend)
                S.op("act", "copy", glr_bf.ap[0:16, :], ps[bglr][0:16, :], r=pk(bglr), w=glr_bf.k())

                S.mm(ps[5][:], wgate_bf[:], glr_bf.ap[0:16, :], True, True, r=["wgate"] + glr_bf.k(),
                     w=pk(5))
                bufA, bufB, bufC = tA[1], tA[2], tA[0]
                S.op("act", "activation", bufA.ap, ps[5][:], AF.Exp, bias=der[:, 0:1], scale=-1.0,
                     r=pk(5) + ["der"], w=bufA.k())
                S.op("act", "activation", bufA.ap, bufA.ap, AF.Ln, bias=epsc[1.0], r=bufA.k() + ["c_eps"],
                     w=bufA.k())
                S.op("dve", "tensor_tensor_scan", bufB.ap, rmask[:], bufA.ap, 0.0, ALU.mult, ALU.add,
                     r=bufA.k() + ["c_rmask"], w=bufB.k())
                ck("gla_a")
                S.op("act", "activation", bufC.ap, bufB.ap, AF.Exp, scale=-1.0 / 16.0, r=bufB.k(),
                     w=bufC.k())
                S.op("act", "activation", bufA.ap, bufB.ap, AF.Exp, scale=1.0 / 16.0, r=bufB.k(),
                     w=bufA.k())
                S.op("dve", "scalar_tensor_tensor", qt.ap, gq_sb.ap, 32.0 ** -0.5, bufC.ap, ALU.mult,
                     ALU.mult, r=gq_sb.k() + bufC.k(), w=qt.k())
                S.op("dve", "tensor_tensor", kt.ap, gk_sb.ap, bufA.ap, ALU.mult, r=gk_sb.k() + bufA.k(),
                     w=kt.k())
                for h in range(4):
                    S.op("dve", "tensor_copy", Qblk[32 * h:32 * h + 32, :, h, :],
                         qt.ap[32 * h:32 * h + 32, :].rearrange("p (c i) -> p c i", i=128),
                         r=qt.k(), w=["Qblk"])
                ck("fm")
                for g in range(4):
                    wbuf, wkey = R_tm.next()
                    for tt in range(4):
                        b = nextbank([1, 2, 3, 4])
                        for kc in range(8):
                            S.mm(ps[b][:, 0:256], hn[:, kc, tt * 128:(tt + 1) * 128], wbuf[:, kc, :],
                                 kc == 0, kc == 7, r=[wkey, ("hn", kc)], w=pk(b, 0, 256))
                        if g == 0:
                            S.op("act", "copy", gv_sb[:, tt, :], ps[b][:, 0:256], r=pk(b, 0, 256),
                                 w=gv_b.k(tt * 256, tt * 256 + 256))
                        elif g == 1:
                            lo = (tt % 2) * 256
                            e1 = tA[5].ap[:, lo:lo + 256]
                            k1 = tA[5].k(lo, lo + 256)
                            kg = gsg_b.k(tt * 256, tt * 256 + 256)
                            S.op("act", "activation", e1, ps[b][:, 0:256], AF.Exp, scale=-1.0,
                                 r=pk(b, 0, 256), w=k1)
                            recip1p(e1, k1)
                            S.op("dve", "tensor_tensor", gsg[:, tt, :], ps[b][:, 0:256], e1, ALU.mult,
                                 r=pk(b, 0, 256) + k1, w=kg)
                            S.op("dve", "tensor_tensor", gsg[:, tt, :], gsg[:, tt, :], g8[:], ALU.mult,
                                 r=kg + ["g8"], w=kg)
                        else:
                            half = g - 2
                            eng = "act" if tt % 2 == 0 else "dve"
                            meth = "copy" if eng == "act" else "tensor_copy"
                            S.op(eng, meth, vS[:, 4 * I + tt, half * 256:(half + 1) * 256],
                                 ps[b][:, 0:256], r=pk(b, 0, 256), w=[("vS", 4 * I + tt, half)])

                ck("tm")
                ck("gla_b")
                trb = psbf(6)

                def emit_kt_transposes():
                    for c in range(4):
                        tr_op(trb[:, c * 128:(c + 1) * 128], kt.ap[:, c * 128:(c + 1) * 128],
                              kt.k() + ["c_ident"], pk(6, 0, 256))
                    S.op("act", "copy", kt_b.ap, trb[:, 0:512], r=pk(6, 0, 256), w=kt_b.k())
                def emit_diag(cc):
                    for w_ in range(31):
                        S.op("dve", "tensor_scalar", dg[:, w_, :], ident[:],
                             pcol_sb[:, PC_CW + cc * 31 + w_:PC_CW + cc * 31 + w_ + 1], None, ALU.mult,
                             r=["c_ident", "pcol"], w=dg_b.k(w_ * 128, w_ * 128 + 128))

                def emit_conv(cc):
                    by = nextbank([2, 3])
                    for w_ in range(31):
                        S.mm(ps[by][:], dg[:, w_, :], zb[:, cc, w_:w_ + T], w_ == 0, w_ == 30,
                             r=dg_b.k(w_ * 128, w_ * 128 + 128) + [("zb", cc)], w=pk(by))
                    S.op("dve", "tensor_copy", zb[:, cc, 0:30], zb[:, cc, T:T + 30], r=[("zb", cc)],
                         w=[("zb", cc)])
                    ysb = (gq_sb, gk_sb)[cc]
                    ybf = tB[2 + cc]
                    cb = pcol_sb[:, PC_CB + cc:PC_CB + cc + 1]
                    S.op("act", "activation", ysb.ap, ps[by][:], AF.Identity, bias=cb,
                         r=pk(by) + ["pcol"], w=ysb.k())
                    S.op("act", "activation", ybf.ap, ps[by][:], AF.Identity, bias=cb,
                         r=pk(by) + ["pcol"], w=ybf.k())

                    def c2():
                        bm = nextbank([2, 3])
                        S.mm(ps[bm][:], blk64[:], ybf.ap, True, True, r=ybf.k() + ["c_blk64"], w=pk(bm))
                        S.op("dve", "tensor_tensor", ysb.ap, ysb.ap, ps[bm][:], ALU.subtract,
                             r=ysb.k() + pk(bm), w=ysb.k())
                        S.op("dve", "tensor_tensor", ybf.ap, ysb.ap, ysb.ap, ALU.mult, r=ysb.k(),
                             w=ybf.k())

                        def c3():
                            bv = nextbank([2, 3])
                            S.mm(ps[bv][:], blk64[:], ybf.ap, True, True, r=ybf.k() + ["c_blk64"],
                                 w=pk(bv))
                            rs = tA[5]
                            rstd_act(rs.ap, rs.k(), ps[bv][:], pk(bv), EPS)
                            S.op("dve", "tensor_tensor", ysb.ap, ysb.ap, rs.ap, ALU.mult,
                                 r=ysb.k() + rs.k(), w=ysb.k())
                            S.op("dve", "tensor_scalar", ysb.ap, ysb.ap,
                                 pcol_sb[:, PC_CG + cc:PC_CG + cc + 1],
                                 pcol_sb[:, PC_CBETA + cc:PC_CBETA + cc + 1], ALU.mult, ALU.add,
                                 r=ysb.k() + ["pcol"], w=ysb.k())
                            S.op("act", "activation", rs.ap, ysb.ap, AF.Exp, scale=-1.0, r=ysb.k(),
                                 w=rs.k())
                            recip1p(rs.ap, rs.k())
                            S.op("dve", "tensor_tensor", mixT[:, 2 + cc, :], ysb.ap, rs.ap, ALU.mult,
                                 r=ysb.k() + rs.k(), w=mixT_b.k((2 + cc) * T, (3 + cc) * T))
                            return None
                        return c3
                    return c2

                ck("gla_c")

                def emit_gla_scores(c):
                    cs_ = slice(c * 128, (c + 1) * 128)
                    S.mm(ps[7][:], kt.ap[:, cs_], Qblk[:, c, :, :].rearrange("p h i -> p (h i)"), True, True,
                         r=kt.k() + ["Qblk"], w=pk(7))
                    scm = glr_bf
                    S.op("dve", "tensor_tensor", scm.ap.rearrange("p (h i) -> p h i", i=128),
                         ps[7][:].rearrange("p (h i) -> p h i", i=128),
                         tri[:].unsqueeze(1).broadcast_to([128, 4, 128]), ALU.mult,
                         r=pk(7) + ["c_tri"], w=scm.k())

                def emit_gla_chunk(c):
                    cs_ = slice(c * 128, (c + 1) * 128)
                    kgv = gv_b.k(c * 256, c * 256 + 256)
                    scm = glr_bf
                    og = og2[c % 2]
                    ck("gla_d")
                    for h in range(4):
                        S.mm(ps[1][32 * h:32 * h + 32, 0:64], kt_tok[:, c, 32 * h:32 * h + 32],
                             gv_sb[:, c, 64 * h:64 * h + 64], True, True,
                             r=kt_b.k() + kgv, w=pk(1), tp=(0, 32 * h))
                    ck("gla_e")
                    for h in range(4):
                        S.mm(ps[0][:, 64 * h:64 * h + 64], scm.ap[:, h * 128:(h + 1) * 128],
                             gv_sb[:, c, 64 * h:64 * h + 64], h == 0, False, r=scm.k() + kgv,
                             w=pk(0, 0, 256), skip=True)
                    S.mm(ps[0][:, 0:256], qt.ap[:, cs_], Sblk[:], False, True, r=qt.k() + ["Sblk"],
                         w=pk(0, 0, 256), skip=True)
                    ck("gla_f")
                    ebl = bufC.ap[:, c * 128 + 127:c * 128 + 128]
                    U = small.ap[:, 0:64]
                    S.op("dve", "tensor_tensor", U, ps[1][:, 0:64], S32[:], ALU.add,
                         r=pk(1) + ["S32"], w=small.k())
                    S.op("dve", "tensor_scalar", S32[:], U, ebl, None, ALU.mult, r=small.k() + bufC.k(),
                         w=["S32"])
                    for h in range(4):
                        S.op("dve", "tensor_scalar", Sblk[32 * h:32 * h + 32, 64 * h:64 * h + 64],
                             U[32 * h:32 * h + 32, :], ebl[32 * h:32 * h + 32, :], None, ALU.mult,
                             r=small.k() + bufC.k(), w=["Sblk"])
                    ck("gla_g")
                    osq_ap = mixT_b.ap[:, 7 * T:8 * T].bitcast(F32)
                    osq_k = mixT_b.k(7 * T, 8 * T)
                    osb_ap = mixT_b.ap[:, 6 * T:7 * T].bitcast(F32)
                    osb_k = mixT_b.k(6 * T, 7 * T)
                    S.op("act", "copy", osb_ap, ps[0][:, 0:256], r=pk(0, 0, 256), w=osb_k)
                    S.op("dve", "tensor_tensor", osq_ap, osb_ap, osb_ap, ALU.mult, r=osb_k, w=osq_k)
                    ss = small.ap[:, 64:68]
                    S.op("dve", "tensor_reduce", ss, osq_ap.rearrange("p (h v) -> p h v", v=64),
                         AX.X, ALU.add, r=osq_k, w=small.k())
                    rstd_act(ss, small.k(), ss, small.k(), 64.0 * EPS)
                    S.op("dve", "tensor_tensor", osq_ap.rearrange("p (h v) -> p h v", v=64),
                         osb_ap.rearrange("p (h v) -> p h v", v=64),
                         ss.unsqueeze(2).broadcast_to([128, 4, 64]), ALU.mult,
                         r=osb_k + small.k(), w=osq_k)
                    S.op("dve", "tensor_tensor", og.ap, osq_ap, gsg[:, c, :], ALU.mult,
                         r=osq_k + gsg_b.k(c * 256, c * 256 + 256), w=og.k())
                    ck("gla_h")

                    def gla_b():
                        for hh in range(2):
                            tr_op(trb[:, 512 + hh * 128:512 + (hh + 1) * 128],
                                  og.ap[:, hh * 128:(hh + 1) * 128], og.k() + ["c_ident"], pk(6, 256, 384))
                        S.op("dve", "tensor_copy", mixT[:, 0:2, cs_],
                             trb[:, 512:768].rearrange("p (a b) -> p a b", b=128),
                             r=pk(6, 256, 384), w=mixT_b.k(0, 2 * T))
                        return None
                    return gla_b

                nkb = 4 * I + 4

                def emit_head(h):
                    kqn = qn_b.k(h * T, (h + 1) * T)

                    def emit_S(j):
                        q0 = 0 if j < 4 * I else (j - 4 * I) * 128
                        for m in range(2):
                            bS = 2 * (j % 2) + m
                            S.mm(ps[bS][:, q0:T], kT[64 * m:64 * m + 64, h, j * 128:(j + 1) * 128],
                                 qn[64 * m:64 * m + 64, h, q0:T], True, True,
                                 r=[("kT", h, j // 4)] + kqn, w=pk(bS, q0, T), tp=(64 * m, 0))

                    def emit_exp(j):
                        q0 = 0 if j < 4 * I else (j - 4 * I) * 128
                        p = j % 2
                        ptb = PTp[p]
                        src = psall[:, (2 * p) * 512:(2 * p + 2) * 512].rearrange("p (m q) -> p m q", q=512)
                        dstv = ptb.ap.rearrange("p (m q) -> p m q", q=512)
                        S.op("act", "activation", dstv[:, :, q0:T], src[:, :, q0:T], AF.Exp, scale=0.125,
                             r=pk(2 * p) + pk(2 * p + 1), w=ptb.k())
                        if j >= 4 * I:
                            S.op("dve", "tensor_tensor", dstv[:, :, q0:q0 + 128], dstv[:, :, q0:q0 + 128],
                                 tri[:].unsqueeze(1).broadcast_to([128, 2, 128]), ALU.mult,
                                 r=ptb.k() + ["c_tri"], w=ptb.k())

                    def emit_AV(j):
                        diag = j >= 4 * I
                        q0 = 0 if not diag else (j - 4 * I) * 128
                        vv = vS[:, j, h * 128:(h + 1) * 128]
                        vk = ("vS", j, h // 2)
                        for m in range(2):
                            ptb = PTp[j % 2]
                            ptap = ptb.ap[:, m * T:(m + 1) * T]
                            if not diag:
                                regions = [(0, T, False)]
                            else:
                                regions = [(q0, q0 + 128, True)]
                                if q0 + 128 < T:
                                    regions.append((q0 + 128, T, False))
                            for ri, (a, b_, last) in enumerate(regions):
                                S.mm(ps[4 + m][:, a:b_], vv, ptap[:, a:b_], j == 0 and ri == 0, last,
                                     r=[vk] + ptb.k(), w=pk(4 + m, a, b_), skip=True)
                            for ri, (a, b_, last) in enumerate(regions):
                                S.mm(ps[6 + m][:, a:b_], ones1[:], ptap[:, a:b_], j == 0 and ri == 0,
                                     last, r=["c_ones1"] + ptb.k(), w=pk(6 + m, a, b_), skip=True)

                    emit_S(0)
                    for j in range(nkb):
                        if j + 1 < nkb:
                            emit_S(j + 1)
                        emit_exp(j)
                        emit_AV(j)
                    r0, r1, o0, o1 = tA[1], tA[2], tA[3 + (h % 2)], tA[5]
                    S.op("act", "activation", r0.ap, ps[6][:], AF.Ln, r=pk(6), w=r0.k())
                    S.op("dve", "tensor_copy", o0.ap, ps[4][:], r=pk(4), w=o0.k())
                    S.op("act", "activation", r1.ap, ps[7][:], AF.Ln, r=pk(7), w=r1.k())
                    S.op("dve", "tensor_copy", o1.ap, ps[5][:], r=pk(5), w=o1.k())
                    osqb = tB[h % 2]

                    def post1b():
                        return _post1b(h, r0, r1, o0, o1, osqb)
                    return post1b

                def _post1b(h, r0, r1, o0, o1, osqb):
                    S.op("act", "activation", r0.ap, r0.ap, AF.Exp, scale=-1.0, r=r0.k(), w=r0.k())
                    S.op("act", "activation", r1.ap, r1.ap, AF.Exp, scale=-1.0, r=r1.k(), w=r1.k())
                    S.op("dve", "tensor_tensor", o0.ap, o0.ap, r0.ap, ALU.mult, r=o0.k() + r0.k(),
                         w=o0.k())
                    S.op("dve", "scalar_tensor_tensor", o1.ap, o1.ap, der[:, 2:3], r1.ap, ALU.mult,
                         ALU.mult, r=o1.k() + r1.k() + ["der"], w=o1.k())
                    S.op("dve", "tensor_tensor", o0.ap, o0.ap, o1.ap, ALU.add, r=o0.k() + o1.k(),
                         w=o0.k())
                    S.op("dve", "tensor_tensor", osqb.ap, o0.ap, o0.ap, ALU.mult, r=o0.k(), w=osqb.k())

                    def head_fin():
                        bs_ = nextbank([2, 3])
                        S.mm(ps[bs_][:], ones128[:], osqb.ap, True, True, r=osqb.k() + ["c_ones128"],
                             w=pk(bs_))
                        rstd_act(r0.ap, r0.k(), ps[bs_][:], pk(bs_), EPS)
                        S.op("dve", "scalar_tensor_tensor", mixT[:, 4 + h, :], o0.ap, der[:, 1:2], r0.ap,
                             ALU.mult, ALU.mult, r=o0.k() + r0.k() + ["der"],
                             w=mixT_b.k((4 + h) * T, (5 + h) * T))
                        return None
                    return head_fin

                pending = []
                for c in range(4):
                    emit_gla_scores(c)
                    if c < 2:
                        emit_diag(c)
                    p1b = emit_head(c)
                    if c == 0:
                        emit_kt_transposes()
                    glb = emit_gla_chunk(c)
                    fin = p1b()
                    cv = emit_conv(c) if c < 2 else None
                    nxt = [f() for f in pending]
                    pending = [fin, glb] + [f for f in nxt if f is not None]
                    if cv is not None:
                        pending.append(cv)
                while pending:
                    pending = [g for g in (f() for f in pending) if g is not None]
                ck("gla")
                ck("conv")
                dbg("mix%d_%d" % (l, I), mixT, [128, 8, T], BF16, r=mixT_b.k())

                ck("attn")
                for j in range(8):
                    wbuf, wkey = R_fm.next()
                    b = nextbank([0, 1])
                    for ki, kc in enumerate((4, 5, 6, 2, 3, 7, 0, 1)):
                        S.mm(ps[b][:], wbuf[:, kc, :], mixT[:, kc, :], ki == 0, ki == 7,
                             r=[wkey] + mixT_b.k(kc * T, (kc + 1) * T), w=pk(b))
                    S.op("dve", "tensor_tensor", xc(j), xc(j), ps[b][:], ALU.add,
                         r=[xk(j)] + pk(b), w=[xk(j)])
                    norm_sq(j)
                    if j >= 2:
                        norm_mm(j - 2, 2, j == 2, False)
                norm_mm(6, 2, False, False)
                norm_mm(7, 2, False, True)

                for c_ in range(8):
                    dbg("xa%d_%d_%d" % (l, I, c_), xc(c_), [128, T], F32, r=[xk(c_)])

                ck("oproj")
                rmsnorm(PC_FFNG, 2, True, stats_done=True)
                rtv = rt.rearrange("p (a b) -> p a b", b=80)
                lg = rtv[:, 0, :].rearrange("p (t n) -> p t n", n=20)
                S.op("dve", "tensor_tensor", lg, ps[3][:, 0:80].rearrange("p (t n) -> p t n", n=20),
                     prow_sb[:, PR_RB:PR_RB + 80].rearrange("p (t n) -> p t n", n=20), ALU.add,
                     r=pk(3, 0, 128) + ["prow"], w=rt_b.k())
                RT = dict(r=rt_b.k(), w=rt_b.k())
                gl = lg[:, :, 0:4]
                gmax = rtv[:, 1, 0:4]
                goh = rtv[:, 1, 4:20].rearrange("p (t n) -> p t n", n=4)
                gd = rtv[:, 1, 20:36].rearrange("p (t n) -> p t n", n=4)
                gsum = rtv[:, 1, 36:40]
                S.op("dve", "tensor_reduce", gmax, gl, AX.X, ALU.max, **RT)
                S.op("dve", "tensor_tensor", goh, gl, gmax.unsqueeze(2).broadcast_to([128, 4, 4]),
                     ALU.is_equal, **RT)
                S.op("dve", "tensor_tensor", gd, gl, gmax.unsqueeze(2).broadcast_to([128, 4, 4]),
                     ALU.subtract, **RT)
                S.op("act", "activation", gd, gd, AF.Exp, **RT)
                S.op("dve", "tensor_reduce", gsum, gd, AX.X, ALU.add, **RT)
                S.op("dve", "reciprocal", gsum, gsum, **RT)
                el = lg[:, :, 4:20].rearrange("p t (g e) -> p t g e", e=4)
                tmp4 = rtv[:, 2, 0:64].rearrange("p (t g e) -> p t g e", g=4, e=4)
                S.op("dve", "tensor_tensor", tmp4, el, goh.unsqueeze(3).broadcast_to([128, 4, 4, 4]),
                     ALU.mult, **RT)
                sel = rtv[:, 3, 0:16].rearrange("p (t e) -> p t e", e=4)
                S.op("dve", "tensor_reduce", sel, tmp4.rearrange("p t g e -> p t e g"), AX.X, ALU.add,
                     **RT)
                m1 = rtv[:, 3, 16:20]
                mk1 = rtv[:, 3, 20:36].rearrange("p (t e) -> p t e", e=4)
                sel2 = rtv[:, 3, 36:52].rearrange("p (t e) -> p t e", e=4)
                m2 = rtv[:, 3, 52:56]
                mk2 = rtv[:, 3, 56:72].rearrange("p (t e) -> p t e", e=4)
                S.op("dve", "tensor_reduce", m1, sel, AX.X, ALU.max, **RT)
                S.op("dve", "tensor_tensor", mk1, sel, m1.unsqueeze(2).broadcast_to([128, 4, 4]),
                     ALU.is_equal, **RT)
                S.op("dve", "scalar_tensor_tensor", sel2, mk1, -1e30, sel, ALU.mult, ALU.add, **RT)
                S.op("dve", "tensor_reduce", m2, sel2, AX.X, ALU.max, **RT)
                S.op("dve", "tensor_tensor", mk2, sel2, m2.unsqueeze(2).broadcast_to([128, 4, 4]),
                     ALU.is_equal, **RT)
                dd = rtv[:, 4, 0:4]
                w1 = rtv[:, 4, 4:8]
                w2 = rtv[:, 4, 8:12]
                S.op("dve", "tensor_tensor", dd, m2, m1, ALU.subtract, **RT)
                S.op("act", "activation", dd, dd, AF.Exp, **RT)
                S.op("dve", "tensor_scalar", w1, dd, 1.0, None, ALU.add, **RT)
                S.op("dve", "reciprocal", w1, w1, **RT)
                S.op("dve", "tensor_tensor", w2, dd, w1, ALU.mult, **RT)
                S.op("dve", "tensor_tensor", w1, w1, gsum, ALU.mult, **RT)
                S.op("dve", "tensor_tensor", w2, w2, gsum, ALU.mult, **RT)
                wl = rtv[:, 4, 12:28].rearrange("p (t e) -> p t e", e=4)
                wl2 = rtv[:, 4, 28:44].rearrange("p (t e) -> p t e", e=4)
                S.op("dve", "tensor_tensor", wl, mk1, w1.unsqueeze(2).broadcast_to([128, 4, 4]),
                     ALU.mult, **RT)
                S.op("dve", "tensor_tensor", wl2, mk2, w2.unsqueeze(2).broadcast_to([128, 4, 4]),
                     ALU.mult, **RT)
                S.op("dve", "tensor_tensor", wl, wl, wl2, ALU.add, **RT)
                gates = rtv[:, 5, 0:64].rearrange("p (t g e) -> p t g e", g=4, e=4)
                S.op("dve", "tensor_tensor", gates, goh.unsqueeze(3).broadcast_to([128, 4, 4, 4]),
                     wl.unsqueeze(2).broadcast_to([128, 4, 4, 4]), ALU.mult, **RT)
                gates_te = rtv[:, 5, 0:64].rearrange("p (t n) -> p t n", n=16)
                dbg("gates%d_%d" % (l, I), rtv[:, 5, 0:64], [128, 64], F32, r=rt_b.k())

                ck("router")
                def emit_G(e):
                    De_e = De[e % 2]
                    S.op("dve", "tensor_tensor", De_e.ap.rearrange("p (c t) -> p c t", t=128),
                         ident[:].unsqueeze(1).broadcast_to([128, 4, 128]),
                         gates_te[:, :, e:e + 1].broadcast_to([128, 4, 128]), ALU.mult,
                         r=rt_b.k() + ["c_ident"], w=De_e.k())
                    bG = 4 + (e % 2)
                    for tt in range(4):
                        S.mm(ps[bG][:, tt * 128:(tt + 1) * 128], ones1[:],
                             De_e.ap[:, tt * 128:(tt + 1) * 128], True, True,
                             r=De_e.k() + ["c_ones1"], w=pk(bG, tt * 128, tt * 128 + 128))

                for e in range(NE):
                    bG = 4 + (e % 2)
                    wg_buf, wg_key = R_gu.next()
                    wu_buf, wu_key = R_gu.next()
                    for hc in range(2):
                        par = (e * 2 + hc) % 2
                        bg_, bu_ = 2 * par, 2 * par + 1
                        for kc in range(8):
                            S.mm(ps[bg_][:], wg_buf[:, hc, kc, :], hn[:, kc, :], kc == 0, kc == 7,
                                 r=[wg_key, ("hn", kc)], w=pk(bg_))
                        for kc in range(8):
                            S.mm(ps[bu_][:], wu_buf[:, hc, kc, :], hn[:, kc, :], kc == 0, kc == 7,
                                 r=[wu_key, ("hn", kc)], w=pk(bu_))
                        if hc == 0 and e == 0:
                            emit_G(0)
                        if hc == 1 and e + 1 < NE:
                            emit_G(e + 1)
                        S.op("act", "activation", sgb[par].ap, ps[bg_][:], AF.Silu, r=pk(bg_),
                             w=sgb[par].k())
                        S.op("dve", "tensor_tensor", hb[par].ap, sgb[par].ap, ps[bu_][:], ALU.mult,
                             r=sgb[par].k() + pk(bu_), w=hb[par].k())
                        eh = e * 2 + hc
                        S.op("dve", "tensor_tensor", actp[:, eh, :], hb[par].ap, ps[bG][:], ALU.mult,
                             r=hb[par].k() + pk(bG), w=actp_b.k(eh * T, (eh + 1) * T))
                for j in DOWN_ORDER:
                    by = 6 + (j % 2)
                    for half in range(2):
                        wbuf, wkey = R_wd.next()
                        for i in range(16):
                            eh = half * 16 + i
                            S.mm(ps[by][:], wbuf[:, i, :], actp[:, eh, :], eh == 0, eh == 31,
                                 r=[wkey] + actp_b.k(eh * T, (eh + 1) * T), w=pk(by))
                    if j < 6:
                        S.op("dve", "tensor_tensor", xc(j), xc(j), ps[by][:], ALU.add,
                             r=[xk(j)] + pk(by), w=[xk(j)])
                        S.dma("pool", dst[j * 128:(j + 1) * 128, t0:t0 + T], xc(j), r=[xk(j)],
                              w=[("xres", l + 1, I, j)])
                    else:
                        tmp = xn32[j - 6]
                        S.op("dve", "tensor_tensor", tmp.ap, xc(j), ps[by][:], ALU.add,
                             r=[xk(j)] + pk(by), w=tmp.k())
                        S.dma("pool", dst[j * 128:(j + 1) * 128, t0:t0 + T], tmp.ap, r=tmp.k(),
                              w=[("xres", l + 1, I, j)])
                    if I + 1 < NB:
                        nl, nI, nsrc = l, I + 1, src
                    elif l + 1 < L:
                        nl, nI, nsrc = l + 1, 0, xmid
                    else:
                        nl = None
                    if nl is not None:
                        nj = []
                        if j >= 2:
                            nj = [j - 2]
                        if j == 2:
                            nj.append(6)
                        if j == 3:
                            nj.append(7)
                        for jn in nj:
                            S.dma("pool", xc(jn, gb + 1), nsrc[jn * 128:(jn + 1) * 128, nI * T:(nI + 1) * T],
                                  r=[("xres", nl, nI, jn)] if nl > 0 else [], w=[xk(jn, gb + 1)])
                ck("moe")

        except _Stop:
            pass
        S.add("sp", None, reads=[("xres", L, I, j) for I in range(NB) for j in range(8)] +
              [("dbg", n) for n in dbg_outs] +
              [("c_wout", l_, 7) for l_ in range(L)] + [("c_wd", l_, 1) for l_ in range(L)])
        S.finalize(st)
        info = dict(ops=S.n_ops, counts=S.max_counts, arena=(mixer_top, moe_top), sbuf_left=nc.sbuf_bytes_remaining,
                    pe_tags=[o.tag for o in S.ops if o.stream == "pe" and o.fn is not None],
                    act_tags=[o.tag for o in S.ops if o.stream == "act" and o.fn is not None and not o.is_dma],
                    dve_tags=[o.tag for o in S.ops if o.stream == "dve" and o.fn is not None and not o.is_dma])
    return nc, info


def prep_weights(inp, L):
    f = np.float32
    w_in = np.asarray(inp["w_in"], f)
    out = {}

    def chunks_kp(w, cols, width):
        res = np.stack([w[:, c:c + width] for c in cols], 0)
        res = res.reshape(len(cols), 8, 128, width).transpose(0, 2, 1, 3)
        return np.ascontiguousarray(res)

    out["w_fm"] = np.stack([chunks_kp(w_in[l], FM_COLS, 128).reshape(14 * 128, 1024) for l in range(L)])
    out["w_glr"] = np.stack([chunks_kp(w_in[l], [512], 16).reshape(128, 128) for l in range(L)])
    out["w_tm"] = np.stack([chunks_kp(w_in[l], TM_COLS, 256).reshape(4 * 128, 2048) for l in range(L)])
    w_o = np.asarray(inp["w_out"], f)
    out["w_out"] = np.stack([chunks_kp(w_o[l], [128 * j for j in range(8)], 128).reshape(8 * 128, 1024)
                             for l in range(L)])
    wg = np.asarray(inp["expert_w_gate"], f)
    wu = np.asarray(inp["expert_w_up"], f)
    wdn = np.asarray(inp["expert_w_down"], f)
    gu = np.stack([wg, wu], 2)
    gu = gu.reshape(L, NE, 2, 8, 128, 2, 128)
    gu = gu.transpose(0, 1, 2, 4, 5, 3, 6)
    out["w_gu"] = np.ascontiguousarray(gu).reshape(L, 32 * 128, 2048)
    wd = wdn.reshape(L, 2, 8, 2, 128, 8, 128)
    wd = wd.transpose(0, 5, 1, 4, 2, 3, 6)
    out["w_d"] = np.ascontiguousarray(wd).reshape(L, 16 * 128, 2048)
    wr = np.concatenate([np.asarray(inp["router_group_w"], f), np.asarray(inp["router_expert_w"], f)], -1)
    wr = wr.reshape(L, 8, 128, 20).transpose(0, 2, 1, 3)
    out["w_r"] = np.ascontiguousarray(wr).reshape(L, 128, 160)
    out["w_gate"] = np.ascontiguousarray(np.asarray(inp["gla_gate_w"], f))
    pc = np.zeros((L, 128, NPC), f)
    pr = np.zeros((L, 128, NPR), f)
    for l in range(L):
        pc[l, :, PC_MIXG:PC_MIXG + 8] = np.asarray(inp["mix_norm_g"][l], f).reshape(8, 128).T
        pc[l, :, PC_FFNG:PC_FFNG + 8] = np.asarray(inp["ffn_norm_g"][l], f).reshape(8, 128).T
        pc[l, :, PC_GB] = np.asarray(inp["gla_gate_b"][l], f)
        pc[l, :, PC_CB:PC_CB + 2] = np.asarray(inp["conv_b"][l], f).reshape(2, 128).T
        pc[l, :, PC_CG:PC_CG + 2] = np.asarray(inp["conv_norm_g"][l], f).reshape(2, 128).T
        pc[l, :, PC_CBETA:PC_CBETA + 2] = np.asarray(inp["conv_norm_b"][l], f).reshape(2, 128).T
        pc[l, :, PC_QG] = np.tile(np.asarray(inp["diff_qnorm_g"][l], f), 2)
        pc[l, :, PC_KG] = np.tile(np.asarray(inp["diff_knorm_g"][l], f), 2)
        pc[l, :, PC_SUB] = np.asarray(inp["diff_subln_g"][l], f)
        cw = np.asarray(inp["conv_w"][l], f)
        pc[l, :, PC_CW:PC_CW + 62] = cw.reshape(31, 2, 128).transpose(2, 1, 0).reshape(128, 62)
        pr[l, :, PR_G8:PR_G8 + 256] = np.tile(np.asarray(inp["gla_norm_g"][l], f), 4)[None, :]
        rb = np.concatenate([np.asarray(inp["router_group_b"][l], f), np.asarray(inp["router_expert_b"][l], f)])
        pr[l, :, PR_RB:PR_RB + 80] = np.tile(rb, 4)[None, :]
        lq = np.concatenate([np.asarray(inp[k][l], f) for k in ("diff_lq1", "diff_lk1", "diff_lq2", "diff_lk2")])
        pr[l, :, PR_LQ:PR_LQ + 256] = lq[None, :]
    out["pcol"] = pc
    out["prow"] = pr
    return out


_CACHE = {}


def run(inputs, SEQ, DEPTH, n_cores, debug=(), trace=False, stop=None):
    key = (SEQ, DEPTH, tuple(debug), stop)
    if key not in _CACHE:
        _CACHE[key] = build(SEQ, DEPTH, debug, stop)
    nc, info = _CACHE[key]
    wts = prep_weights(inputs, DEPTH)
    x = np.asarray(inputs["x"], np.float32)
    in_maps = []
    for c in range(n_cores):
        m = dict(wts)
        m["xT"] = np.ascontiguousarray(x[c].T)
        in_maps.append(m)
    res = run_bass_kernel_spmd(nc, in_maps, core_ids=list(range(n_cores)), trace=trace)
    y = np.stack([np.asarray(r["yT"]).T for r in res.results], 0)
    return np.ascontiguousarray(y.astype(np.float32)), res, info


def kernel(**inputs):
    x = np.asarray(inputs["x"])
    B, SEQ, _ = x.shape
    DEPTH = np.asarray(inputs["w_in"]).shape[0]
    y, _, _ = run(inputs, SEQ, DEPTH, B)
    return y
```

```python
import contextlib
import math
import numpy as np
import concourse.bass as bass
import concourse.mybir as mybir
from concourse.bass_utils import run_bass_kernel_spmd

F32 = mybir.dt.float32
BF16 = mybir.dt.bfloat16
ALU = mybir.AluOpType
AF = mybir.ActivationFunctionType
AX = mybir.AxisListType

COMPUTE = ("pe", "act", "dve", "pool")
STREAMS = ("pe", "act", "dve", "pool", "sp")


class _Op:
    __slots__ = ("stream", "fn", "reads", "writes", "is_dma", "idx", "sidx", "signal",
                 "deps", "count", "dsem", "dval", "dprev", "tag")

    def __init__(self, stream, fn, reads, writes, is_dma):
        self.stream = stream
        self.fn = fn
        self.reads = reads
        self.writes = writes
        self.is_dma = is_dma
        self.signal = is_dma
        self.deps = []
        self.count = None
        self.dsem = None
        self.dval = None
        self.dprev = None


def _flat(keys):
    out = []
    for k in keys:
        if isinstance(k, list):
            out.extend(_flat(k))
        else:
            out.append(k)
    return out


class Sched:
    def __init__(self, nc, n_dma_sems=8):
        self.nc = nc
        self.ops = []
        self.n_dma_sems = n_dma_sems
        self.tag = "init"
        self.eng = {"pe": nc.tensor, "act": nc.scalar, "dve": nc.vector, "pool": nc.gpsimd,
                    "sp": nc.sync}

    def add(self, stream, fn, reads=(), writes=(), dma=False):
        op = _Op(stream, fn, tuple(_flat(reads)), tuple(_flat(writes)), dma)
        op.tag = self.tag
        op.idx = len(self.ops)
        self.ops.append(op)
        return op

    def op(self, stream, method, *args, r=(), w=(), **kw):
        return self.add(stream, lambda e: getattr(e, method)(*args, **kw), r, w)

    def dma(self, stream, out, in_, r=(), w=(), **kw):
        return self.add(stream, lambda e: e.dma_start(out=out, in_=in_, **kw), r, w, dma=True)

    def mm(self, out, lhsT, rhs, start, stop, r=(), w=(), tp=None, skip=False):
        kw = {}
        if tp is not None:
            kw["tile_position"] = tp
        if skip:
            kw["skip_group_check"] = True
        fn = lambda e: e.matmul(out, lhsT, rhs, start=start, stop=stop, **kw)
        op = self.add("pe", fn, r, w)
        op.tag = (op.tag, "%d*%d*%d" % (lhsT.shape[0], lhsT.shape[1], rhs.shape[-1] if len(rhs.shape) == 2
                                          else int(np.prod(rhs.shape[1:]))))
        return op

    def finalize(self, stack):
        nc = self.nc
        ops = self.ops
        last_w = {}
        readers = {}
        for op in ops:
            deps = set()
            for k in op.reads:
                w = last_w.get(k)
                if w is not None:
                    deps.add((w, "raw"))
            for k in op.writes:
                w = last_w.get(k)
                if w is not None:
                    deps.add((w, "waw"))
                for r in readers.get(k, ()):
                    deps.add((r, "war"))
            for k in op.reads:
                readers.setdefault(k, []).append(op.idx)
            for k in op.writes:
                last_w[k] = op.idx
                readers[k] = []
            best = {}
            dma_deps = set()
            for (d, kind) in deps:
                if d == op.idx:
                    continue
                p = ops[d]
                if p.is_dma:
                    dma_deps.add(d)
                    continue
                if p.stream == op.stream and not op.is_dma:
                    if p.stream == "pe":
                        continue
                    if kind != "raw":
                        continue
                if best.get(p.stream, -1) < d:
                    best[p.stream] = d
            op.deps = sorted(best.values()) + sorted(dma_deps)
        known = {s: {} for s in STREAMS}
        cnt = {s: 0 for s in STREAMS}
        for op in ops:
            cnt[op.stream] += 1
            op.sidx = cnt[op.stream]
        for op in ops:
            kept = []
            kn = known[op.stream]
            for d in op.deps:
                p = ops[d]
                if p.is_dma:
                    key = ("dma", d)
                    if key in kn:
                        continue
                    kn[key] = True
                    kept.append(d)
                else:
                    if kn.get(p.stream, 0) >= p.sidx:
                        continue
                    kn[p.stream] = p.sidx
                    kept.append(d)
                    p.signal = True
            op.deps = kept
        sems = {}
        for s in COMPUTE:
            sems[s] = stack.enter_context(nc.semaphore("sem_" + s))
        c = {s: 0 for s in COMPUTE}
        dma_pool = {}
        dma_state = {}
        for op in ops:
            if op.is_dma:
                pool = dma_pool.get(op.stream)
                if pool is None:
                    pool = [stack.enter_context(nc.semaphore("dsem_%s_%d" % (op.stream, i)))
                            for i in range(self.n_dma_sems)]
                    dma_pool[op.stream] = pool
                    dma_state[op.stream] = [0, [0] * self.n_dma_sems]
                st = dma_state[op.stream]
                i = st[0] % self.n_dma_sems
                st[0] += 1
                op.dsem = pool[i]
                op.dprev = st[1][i]
                st[1][i] += 16
                op.dval = st[1][i]
            elif op.signal:
                c[op.stream] += 1
                op.count = c[op.stream]
        self.max_counts = dict(c)
        self.n_ops = dict(cnt)
        assert max(c.values()) < 60000, c
        for op in ops:
            e = self.eng[op.stream]
            for d in op.deps:
                p = ops[d]
                if p.is_dma:
                    e.wait_ge(p.dsem, p.dval)
                else:
                    e.wait_ge(sems[p.stream], p.count)
            if op.is_dma:
                if op.dprev > 0:
                    e.wait_ge(op.dsem, op.dprev)
                ins = op.fn(e)
                ins.then_inc(op.dsem, 16)
            else:
                if op.fn is None:
                    continue
                ins = op.fn(e)
                if op.signal:
                    ins.then_inc(sems[op.stream], 1)
        return self


D = 1024
T = 512
NIN = 2832
EPS = 1e-6
NE = 16
FM_COLS = [0, 128, 784, 912, 1040, 1168] + [1296 + 128 * h for h in range(4)] + \
          [1808 + 128 * h for h in range(4)]
FM_GQ, FM_GK, FM_A0, FM_A1, FM_G0, FM_G1, FM_DQ, FM_DK = 0, 1, 2, 3, 4, 5, 6, 10
TM_COLS = [256, 528, 2320, 2576]
PC_MIXG, PC_FFNG, PC_GB, PC_CB, PC_CG, PC_CBETA, PC_QG, PC_KG, PC_SUB, PC_CW = 0, 8, 16, 17, 19, 21, 23, 24, 25, 26
NPC = 26 + 62
PR_G8, PR_RB, PR_LQ = 0, 256, 336
NPR = 336 + 256


def lambda_init(l):
    return 0.8 - 0.6 * math.exp(-0.3 * l)


class _Stop(Exception):
    pass


def build(SEQ, DEPTH, debug=(), stop=None):
    NB = SEQ // T
    NT = SEQ // 128
    nc = bass.Bass("TRN2", target_bir_lowering=False)
    L = DEPTH

    def din(name, shape, dt=F32):
        return nc.dram_tensor(name, shape, dt, kind="ExternalInput").ap()

    def dint(name, shape, dt):
        return nc.dram_tensor(name, shape, dt, kind="Internal").ap()

    xT_d = din("xT", [D, SEQ])
    w_fm = din("w_fm", [L, 14 * 128, 1024])
    w_glr = din("w_glr", [L, 128, 128])
    w_tm = din("w_tm", [L, 4 * 128, 2048])
    w_out = din("w_out", [L, 8 * 128, 1024])
    w_gu = din("w_gu", [L, 32 * 128, 2048])
    w_d = din("w_d", [L, 16 * 128, 2048])
    w_r = din("w_r", [L, 128, 160])
    w_gate = din("w_gate", [L, 16, 128])
    pcol = din("pcol", [L, 128, NPC])
    prow = din("prow", [L, 128, NPR])
    w_fm_b = dint("w_fm_b", [L, 14 * 128, 1024], BF16)
    w_glr_b = dint("w_glr_b", [L, 128, 128], BF16)
    w_tm_b = dint("w_tm_b", [L, 4 * 128, 2048], BF16)
    w_out_b = dint("w_out_b", [L, 8 * 128, 1024], BF16)
    w_gu_b = dint("w_gu_b", [L, 32 * 128, 2048], BF16)
    w_d_b = dint("w_d_b", [L, 16 * 128, 2048], BF16)
    xmid = dint("xmid", [D, SEQ], F32)
    yT_d = nc.dram_tensor("yT", [D, SEQ], F32, kind="ExternalOutput").ap()
    dbg_outs = {}

    st = contextlib.ExitStack()
    with st:
        S = Sched(nc)

        def sb(name, shape, dt):
            return st.enter_context(nc.sbuf_tensor(name, shape, dt))

        psall = st.enter_context(nc.psum_tensor("psall", [128, 4096], F32))

        class _Bank:
            def __init__(self, b):
                self.b = b

            def __getitem__(self, key):
                return psall[:, self.b * 512:(self.b + 1) * 512][key]

        ps = [_Bank(b) for b in range(8)]

        def pk(b, lo=0, hi=512):
            return [("ps", b)]

        def psbf(b):
            return psall[:, b * 512:(b + 1) * 512].bitcast(BF16)

        kT = sb("kT", [128, 4, SEQ], BF16)
        vS = sb("vS", [128, NT, 512], BF16)
        NSLOT = 10
        xT = sb("xT_sb", [128, NSLOT, T], F32)
        hn = sb("hn", [128, 8, T], BF16)
        ident = sb("ident", [128, 128], BF16)
        tri = sb("tri", [128, 128], BF16)
        ones1 = sb("ones1", [128, 128], BF16)
        onesD = sb("onesD", [128, 128], BF16)
        ones128 = sb("ones128", [128, 128], BF16)
        blk64 = sb("blk64", [128, 128], BF16)
        rmask = sb("rmask", [128, T], F32)
        cst = sb("cst", [128, 4], F32)
        pcol_sb = sb("pcol_sb", [128, NPC], F32)
        prow_sb = sb("prow_sb", [128, NPR], F32)
        wr_sb = sb("wr_sb", [128, 160], F32)
        wgate_bf = sb("wgate_bf", [16, 128], BF16)
        wgate_f = sb("wgate_f", [16, 128], F32)
        wglr_sb = sb("wglr_sb", [128, 128], BF16)
        der = sb("der", [128, 8], F32)
        g8 = sb("g8", [128, 256], F32)
        S32 = sb("S32", [128, 64], F32)
        Sblk = sb("Sblk", [128, 256], BF16)
        Qblk = sb("Qblk", [128, 4, 4, 128], BF16)
        zb = sb("zb", [128, 2, 30 + T], BF16)
        lamt = sb("lamt", [128, 128], F32)
        NFM, NTM, NGU, NWD = 4, 2, 4, 3
        wfm_r = [sb("wfm_r%d" % i, [128, 8, 128], BF16) for i in range(NFM)]
        wtm_r = [sb("wtm_r%d" % i, [128, 8, 256], BF16) for i in range(NTM)]
        wgu_r = [sb("wgu_r%d" % i, [128, 2, 8, 128], BF16) for i in range(NGU)]
        wd_r = [sb("wd_r%d" % i, [128, 16, 128], BF16) for i in range(NWD)]
        ARENA = 14080 + 128
        arena = sb("arena", [128, ARENA], F32)

        GR = 128

        class Buf:
            def __init__(self, ap, base, dt, cols):
                self.ap, self.base, self.dt, self.cols = ap, base, dt, cols
                self.per = 2 if dt == BF16 else 1

            def k(self, lo=0, hi=None):
                hi = self.cols if hi is None else hi
                a = self.base + lo // self.per
                b = self.base + (hi + self.per - 1) // self.per
                return [("ar", g) for g in range(a // GR, (b + GR - 1) // GR)]

        class Arena:
            def __init__(self):
                self.off = 0

            def _take(self, c32):
                c32 = ((c32 + GR - 1) // GR) * GR
                a = self.off
                self.off += c32
                assert self.off <= ARENA, self.off
                return a, c32

            def f32(self, cols):
                a, c32 = self._take(cols)
                return Buf(arena[:, a:a + cols], a, F32, cols)

            def bf16(self, cols):
                a, c32 = self._take((cols + 1) // 2)
                return Buf(arena[:, a:a + (cols + 1) // 2].bitcast(BF16), a, BF16, cols)

        A = Arena()
        mixT_b = A.bf16(8 * T)
        mixT = mixT_b.ap.rearrange("p (c t) -> p c t", t=T)
        gq_sb = A.f32(T)
        gk_sb = A.f32(T)
        glr_bf = A.bf16(T)
        gv_b = A.bf16(4 * 256)
        gv_sb = gv_b.ap.rearrange("p (c t) -> p c t", t=256)
        gsg_b = A.f32(4 * 256)
        gsg = gsg_b.ap.rearrange("p (c t) -> p c t", t=256)
        qn_b = A.bf16(4 * T)
        qn = qn_b.ap.rearrange("p (c t) -> p c t", t=T)
        tA = [A.f32(T) for _ in range(6)]
        tB = [A.bf16(T) for _ in range(4)]
        PTp = [A.bf16(2 * T) for _ in range(2)]
        qt = A.bf16(T)
        kt = A.bf16(T)
        kt_b = A.bf16(4 * 128)
        kt_tok = kt_b.ap.rearrange("p (c t) -> p c t", t=128)
        og2 = [A.bf16(256) for _ in range(2)]
        small = A.f32(128)
        dg_b = A.bf16(31 * 128)
        dg = dg_b.ap.rearrange("p (c t) -> p c t", t=128)
        mixer_top = A.off
        M = Arena()
        actp_b = M.bf16(32 * T)
        actp = actp_b.ap.rearrange("p (c t) -> p c t", t=T)
        xn32 = [M.f32(T) for _ in range(2)]
        sgb = [M.f32(T) for _ in range(2)]
        hb = [M.f32(T) for _ in range(2)]
        De = [M.bf16(4 * 128) for _ in range(2)]
        rt_b = M.f32(640)
        rt = rt_b.ap
        moe_top = M.off

        def dbg(name, ap, shape, dt=F32, r=()):
            if name not in debug:
                return
            t = nc.dram_tensor("dbg_" + name, list(shape), dt, kind="ExternalOutput").ap()
            dbg_outs[name] = t
            S.dma("sp", t, ap, r=list(r), w=[("dbg", name)])

        S.op("pool", "memset", ones1[:], 1.0, w=["c_ones1"])
        S.op("pool", "memset", onesD[:], 1.0 / 1024.0, w=["c_onesD"])
        S.op("pool", "memset", ones128[:], 1.0 / 128.0, w=["c_ones128"])
        S.op("pool", "memset", ident[:], 1.0, w=["c_ident"])
        S.op("pool", "affine_select", ident[:], ident[:], [[-1, 128]], ALU.is_equal, 0.0, base=0,
             channel_multiplier=1, r=["c_ident"], w=["c_ident"])
        S.op("pool", "memset", tri[:], 1.0, w=["c_tri"])
        S.op("pool", "affine_select", tri[:], tri[:], [[1, 128]], ALU.is_ge, 0.0, base=0,
             channel_multiplier=-1, r=["c_tri"], w=["c_tri"])
        S.op("pool", "memset", blk64[:], 0.0, w=["c_blk64"])
        S.op("pool", "memset", blk64[0:64, 0:64], 1.0 / 64.0, r=["c_blk64"], w=["c_blk64"])
        S.op("pool", "memset", blk64[64:128, 64:128], 1.0 / 64.0, r=["c_blk64"], w=["c_blk64"])
        S.op("pool", "memset", Qblk[:].rearrange("p a b c -> p (a b c)"), 0.0, w=["Qblk"])
        S.op("pool", "memset", rmask[:], 1.0, w=["c_rmask"])
        S.op("pool", "memset", rmask[:].rearrange("p (c t) -> p c t", t=128)[:, :, 0:1], 0.0,
             r=["c_rmask"], w=["c_rmask"])

        S.op("pool", "memset", cst[:, 0:1], EPS, w=["c_eps"])
        S.op("pool", "memset", cst[:, 1:2], 64.0 * EPS, r=["c_eps"], w=["c_eps"])
        S.op("pool", "memset", cst[:, 2:3], 1.0, r=["c_eps"], w=["c_eps"])
        S.op("pool", "memset", cst[:, 3:4], 0.0, r=["c_eps"], w=["c_eps"])
        epsc = {EPS: cst[:, 0:1], 64.0 * EPS: cst[:, 1:2], 1.0: cst[:, 2:3]}

        def flat128(ap):
            return ap.rearrange("(p a) c -> p (a c)", p=128)

        for c_ in range(8):
            S.dma("sp", xT[:, c_, :], xT_d[c_ * 128:(c_ + 1) * 128, 0:T], w=[("xT", c_)])

        def emit_casts(l):
            for j in range(14):
                S.dma("pool", w_fm_b[l, j * 128:(j + 1) * 128, :], w_fm[l, j * 128:(j + 1) * 128, :],
                      r=[("xT", c_) for c_ in range(8)] if (l == 0 and j == 2) else [],
                      w=[("c_wfm", l, j)])
                if j == 0:
                    S.dma("pool", w_glr_b[l], w_glr[l], w=[("c_wglr", l)])
            for j in range(4):
                S.dma("pool", w_tm_b[l, j * 128:(j + 1) * 128, :], w_tm[l, j * 128:(j + 1) * 128, :],
                      w=[("c_wtm", l, j)])
            for j in range(8):
                S.dma("pool", w_out_b[l, j * 128:(j + 1) * 128, :], w_out[l, j * 128:(j + 1) * 128, :],
                      w=[("c_wout", l, j)])
            for g in range(4):
                S.dma("pool", flat128(w_gu_b[l, g * 1024:(g + 1) * 1024]),
                      flat128(w_gu[l, g * 1024:(g + 1) * 1024]),
                      r=[("c_wfm", l, 13), ("c_wtm", l, 3), ("c_wout", l, 7)] if g == 0
                      else [("c_wgu", l, g - 1)],
                      w=[("c_wgu", l, g)])
            for g in range(2):
                S.dma("pool", flat128(w_d_b[l, g * 1024:(g + 1) * 1024]),
                      flat128(w_d[l, g * 1024:(g + 1) * 1024]),
                      r=[("c_wgu", l, 1)] if g == 0 else [("c_wd", l, 0), ("c_wgu", l, 3)],
                      w=[("c_wd", l, g)])

        emit_casts(0)

        class Ring:
            def __init__(self, name, bufs, items, loader, hold=1):
                self.name, self.bufs, self.items, self.loader = name, bufs, items, loader
                self.hold = hold
                self.n_loaded = 0
                self.n_used = 0

            def key(self, i):
                return (self.name, i % len(self.bufs))

            def prefetch(self, upto):
                upto = min(upto, len(self.items))
                while self.n_loaded < upto:
                    i = self.n_loaded
                    self.loader(self.bufs[i % len(self.bufs)], self.items[i], self.key(i))
                    self.n_loaded += 1

            def next(self):
                i = self.n_used
                self.prefetch(i + len(self.bufs) - (self.hold - 1))
                self.n_used += 1
                return self.bufs[i % len(self.bufs)], self.key(i)

        DOWN_ORDER = (2, 3, 4, 5, 6, 7, 0, 1)
        fm_items = []
        tm_items = []
        gu_items = []
        wd_items = []
        for l in range(L):
            for I in range(NB):
                for j in range(14):
                    fm_items.append(("fm", l, j))
                for j in range(8):
                    fm_items.append(("out", l, j))
                for g in range(4):
                    tm_items.append((l, g))
                for e in range(NE):
                    for gu in range(2):
                        gu_items.append((l, e, gu))
                for j in DOWN_ORDER:
                    for half in range(2):
                        wd_items.append((l, j, half))

        def load_fm(buf, it, key):
            kind, l, j = it
            if kind == "fm":
                S.dma("sp", buf[:].rearrange("p a b -> p (a b)"), w_fm_b[l, j * 128:(j + 1) * 128, :],
                      r=[("c_wfm", l, j)], w=[key])
            else:
                S.dma("sp", buf[:].rearrange("p a b -> p (a b)"), w_out_b[l, j * 128:(j + 1) * 128, :],
                      r=[("c_wout", l, j)], w=[key])

        def load_tm(buf, it, key):
            l, g = it
            S.dma("sp", buf[:].rearrange("p a b -> p (a b)"), w_tm_b[l, g * 128:(g + 1) * 128, :],
                  r=[("c_wtm", l, g)], w=[key])

        def load_gu(buf, it, key):
            l, e, gu = it
            row = (e * 2 + gu) * 128
            S.dma("sp", buf[:].rearrange("p a b c -> p (a b c)"), w_gu_b[l, row:row + 128, :],
                  r=[("c_wgu", l, e // 4)], w=[key])

        def load_wd(buf, it, key):
            l, j, half = it
            row = (j * 2 + half) * 128
            S.dma("sp", buf[:].rearrange("p a b -> p (a b)"), w_d_b[l, row:row + 128, :],
                  r=[("c_wd", l, j // 4)], w=[key])

        R_fm = Ring("r_fm", wfm_r, fm_items, load_fm)
        R_tm = Ring("r_tm", wtm_r, tm_items, load_tm)
        R_gu = Ring("r_gu", wgu_r, gu_items, load_gu, hold=2)
        R_wd = Ring("r_wd", wd_r, wd_items, load_wd)

        bank_rr = [0]

        def nextbank(choices):
            b = choices[bank_rr[0] % len(choices)]
            bank_rr[0] += 1
            return b

        ev = [0]

        def alt(engs=("dve", "pool")):
            ev[0] += 1
            return engs[ev[0] % len(engs)]

        def rstd_act(dst, dkeys, src, skeys, eps):
            S.op("act", "activation", dst, src, AF.Ln, bias=epsc[eps], r=skeys + ["c_eps"], w=dkeys)
            S.op("act", "activation", dst, dst, AF.Exp, scale=-0.5, r=dkeys, w=dkeys)

        def recip1p(dst, dkeys):
            S.op("act", "activation", dst, dst, AF.Ln, bias=epsc[1.0], r=dkeys + ["c_eps"], w=dkeys)
            S.op("act", "activation", dst, dst, AF.Exp, scale=-1.0, r=dkeys, w=dkeys)

        def ck(name):
            S.tag = "post_" + name
            if stop == name:
                raise _Stop()

        def tr_op(o, i_, r, w):
            S.add("pe", lambda e: e.transpose(o, i_, ident[:]), r, w)

        try:
          ck("casts")
          for l in range(L):
            src = xT_d if l == 0 else xmid
            dst = yT_d if l == L - 1 else xmid
            li = lambda_init(l)
            S.dma("sp", pcol_sb[:], pcol[l], w=["pcol"])
            S.dma("sp", prow_sb[:], prow[l], w=["prow"])
            S.dma("sp", wr_sb[:], w_r[l], w=["wr"])
            S.dma("sp", wgate_f[:], w_gate[l], w=["wgate_f"])
            S.op("dve", "tensor_copy", wgate_bf[:], wgate_f[:], r=["wgate_f"], w=["wgate"])
            S.dma("sp", wglr_sb[:], w_glr_b[l], r=[("c_wglr", l)], w=["wglr"])
            S.op("dve", "tensor_scalar", der[:, 0:1], pcol_sb[:, PC_GB:PC_GB + 1], -1.0, None, ALU.mult,
                 r=["pcol"], w=["der"])
            S.op("dve", "tensor_scalar", der[:, 1:2], pcol_sb[:, PC_SUB:PC_SUB + 1], 1.0 - li, None,
                 ALU.mult, r=["pcol"], w=["der"])
            S.op("dve", "tensor_scalar", g8[:], prow_sb[:, PR_G8:PR_G8 + 256], 8.0, None, ALU.mult,
                 r=["prow"], w=["g8"])
            S.op("dve", "tensor_tensor", lamt[:, 0:64], prow_sb[:, PR_LQ:PR_LQ + 64],
                 prow_sb[:, PR_LQ + 64:PR_LQ + 128], ALU.mult, r=["prow"], w=["lamt"])
            S.op("dve", "tensor_tensor", lamt[:, 64:128], prow_sb[:, PR_LQ + 128:PR_LQ + 192],
                 prow_sb[:, PR_LQ + 192:PR_LQ + 256], ALU.mult, r=["prow", "lamt"], w=["lamt"])
            S.op("dve", "tensor_reduce", der[:, 4:6], lamt[:].rearrange("p (a b) -> p a b", b=64),
                 AX.X, ALU.add, r=["lamt"], w=["der"])
            S.op("act", "activation", der[:, 6:8], der[:, 4:6], AF.Exp, r=["der"], w=["der"])
            S.op("dve", "tensor_tensor", der[:, 2:3], der[:, 7:8], der[:, 6:7], ALU.subtract,
                 r=["der"], w=["der"])
            S.op("dve", "tensor_scalar", der[:, 2:3], der[:, 2:3], -li, None, ALU.add,
                 r=["der"], w=["der"])
            S.op("dve", "memset", S32[:], 0.0, w=["S32"])
            S.op("dve", "memset", Sblk[:], 0.0, w=["Sblk"])
            S.op("dve", "memset", zb[:, :, 0:30], 0.0, w=[("zb", 0), ("zb", 1)])

            ck("params")
            for I in range(NB):
                t0 = I * T
                gb = l * NB + I

                def xslot(j, g=None):
                    return (j + 2 * (gb if g is None else g)) % NSLOT

                def xc(j, g=None):
                    return xT[:, xslot(j, g), :]

                def xk(j, g=None):
                    return ("xT", xslot(j, g))

                if l + 1 < L and I == min(1, NB - 1):
                    emit_casts(l + 1)

                def norm_sq(kc):
                    S.op("act", "activation", hn[:, kc, :], xc(kc), AF.Square,
                         r=[xk(kc)], w=[("hn", kc)])

                def norm_mm(kc, bank, first, last):
                    S.mm(ps[bank][:], onesD[:], hn[:, kc, :], first, last,
                         r=[("hn", kc), "c_onesD"], w=pk(bank))

                def rmsnorm(gbase, bank, router, stats_done=False):
                    order = (6, 7, 0, 1, 2, 3, 4, 5)
                    if not stats_done:
                        for kc in order:
                            S.op("act", "activation", hn[:, kc, :], xc(kc), AF.Square,
                                 r=[xk(kc)], w=[("hn", kc)])
                        for ki, kc in enumerate(order):
                            S.mm(ps[bank][:], onesD[:], hn[:, kc, :], ki == 0, ki == 7,
                                 r=[("hn", kc), "c_onesD"], w=pk(bank))
                    rstd = tA[0]
                    rstd_act(rstd.ap, rstd.k(), ps[bank][:], pk(bank), EPS)
                    for kc in range(8):
                        gc = pcol_sb[:, gbase + kc:gbase + kc + 1]
                        if not router:
                            S.op("dve", "scalar_tensor_tensor", hn[:, kc, :], xc(kc), gc, rstd.ap,
                                 ALU.mult, ALU.mult, r=[xk(kc), rstd.k(), "pcol"], w=[("hn", kc)])
                        else:
                            xb = xn32[kc % 2]
                            S.op("dve", "scalar_tensor_tensor", xb.ap, xc(kc), gc, rstd.ap,
                                 ALU.mult, ALU.mult, r=[xk(kc), rstd.k(), "pcol"], w=xb.k())
                            S.op("act", "copy", hn[:, kc, :], xb.ap, r=xb.k(), w=[("hn", kc)])
                            for tt in range(4):
                                S.mm(ps[3][:, tt * 20:(tt + 1) * 20], xb.ap[:, tt * 128:(tt + 1) * 128],
                                     wr_sb[:, kc * 20:(kc + 1) * 20], kc == 0 and tt == 0,
                                     kc == 7 and tt == 3, r=[xb.k(), "wr"], w=pk(3, 0, 128), skip=True)

                ck("load")
                rmsnorm(PC_MIXG, 0, False)
                ck("norm1")
                dbg("hn%d_%d" % (l, I), hn[:], [128, 8, T], BF16, r=[("hn", c) for c in range(8)])

                def fm_chunk():
                    wbuf, wkey = R_fm.next()
                    b = nextbank([0, 1, 2, 3, 4, 7])
                    for kc in range(8):
                        S.mm(ps[b][:], wbuf[:, kc, :], hn[:, kc, :], kc == 0, kc == 7,
                             r=[wkey, ("hn", kc)], w=pk(b))
                    return b

                b = fm_chunk()
                S.op("dve", "tensor_copy", gq_sb.ap, ps[b][:], r=pk(b), w=gq_sb.k())
                b = fm_chunk()
                S.op("dve", "tensor_copy", gk_sb.ap, ps[b][:], r=pk(b), w=gk_sb.k())
                ba = [fm_chunk(), fm_chunk()]
                for cc in range(2):
                    bg = fm_chunk()
                    tt_ = tA[1 + cc]
                    S.op("act", "activation", tt_.ap, ps[bg][:], AF.Exp, scale=-1.0, r=pk(bg), w=tt_.k())
                    recip1p(tt_.ap, tt_.k())
                    S.op("dve", "tensor_tensor", zb[:, cc, 30:30 + T], ps[ba[cc]][:], tt_.ap,
                         ALU.mult, r=pk(ba[cc]) + tt_.k(), w=[("zb", cc)])
                pend = None

                def qk_finish(p):
                    b, is_k, h, sqb = p
                    b2 = nextbank([5, 6])
                    S.mm(ps[b2][:], blk64[:], sqb.ap, True, True, r=[sqb.k(), "c_blk64"], w=pk(b2))
                    rs = tA[3 + (h % 2)]
                    rstd_act(rs.ap, rs.k(), ps[b2][:], pk(b2), EPS)
                    pc_ = PC_KG if is_k else PC_QG
                    gcol = pcol_sb[:, pc_:pc_ + 1]
                    if is_k:
                        S.op("dve", "scalar_tensor_tensor", kT[:, h, t0:t0 + T], ps[b][:], gcol, rs.ap,
                             ALU.mult, ALU.mult, r=pk(b) + rs.k() + ["pcol"], w=[("kT", h, I)])
                    else:
                        S.op("dve", "scalar_tensor_tensor", qn[:, h, :], ps[b][:], gcol, rs.ap,
                             ALU.mult, ALU.mult, r=pk(b) + rs.k() + ["pcol"],
                             w=qn_b.k(h * T, (h + 1) * T))

                for is_k in (False, True):
                    for h in range(4):
                        b = fm_chunk()
                        sqb = tB[(h % 2) + (2 if is_k else 0)]
                        S.op("act", "activation", sqb.ap, ps[b][:], AF.Square, r=pk(b), w=sqb.k())
                        if pend is not None:
                            qk_finish(pend)
                        pend = (b, is_k, h, sqb)
                bglr = nextbank([1, 2, 3, 4])
                for kc in range(8):
                    S.mm(ps[bglr][0:16, :], wglr_sb[:, kc * 16:(kc + 1) * 16], hn[:, kc, :], kc == 0,
                         kc == 7, r=["wglr", ("hn", kc)], w=pk(bglr))
                qk_finish(pend)
                S.op("act", "copy", glr_bf.ap[0:16, :], ps[bglr][0:16, :], r=pk(bglr), w=glr_bf.k())

                S.mm(ps[5][:], wgate_bf[:], glr_bf.ap[0:16, :], True, True, r=["wgate"] + glr_bf.k(),
                     w=pk(5))
                bufA, bufB, bufC = tA[1], tA[2], tA[0]
                S.op("act", "activation", bufA.ap, ps[5][:], AF.Exp, bias=der[:, 0:1], scale=-1.0,
                     r=pk(5) + ["der"], w=bufA.k())
                S.op("act", "activation", bufA.ap, bufA.ap, AF.Ln, bias=epsc[1.0], r=bufA.k() + ["c_eps"],
                     w=bufA.k())
                S.op("dve", "tensor_tensor_scan", bufB.ap, rmask[:], bufA.ap, 0.0, ALU.mult, ALU.add,
                     r=bufA.k() + ["c_rmask"], w=bufB.k())
                ck("gla_a")
                S.op("act", "activation", bufC.ap, bufB.ap, AF.Exp, scale=-1.0 / 16.0, r=bufB.k(),
                     w=bufC.k())
                S.op("act", "activation", bufA.ap, bufB.ap, AF.Exp, scale=1.0 / 16.0, r=bufB.k(),
                     w=bufA.k())
                S.op("dve", "scalar_tensor_tensor", qt.ap, gq_sb.ap, 32.0 ** -0.5, bufC.ap, ALU.mult,
                     ALU.mult, r=gq_sb.k() + bufC.k(), w=qt.k())
                S.op("dve", "tensor_tensor", kt.ap, gk_sb.ap, bufA.ap, ALU.mult, r=gk_sb.k() + bufA.k(),
                     w=kt.k())
                for h in range(4):
                    S.op("dve", "tensor_copy", Qblk[32 * h:32 * h + 32, :, h, :],
                         qt.ap[32 * h:32 * h + 32, :].rearrange("p (c i) -> p c i", i=128),
                         r=qt.k(), w=["Qblk"])
                ck("fm")
                for g in range(4):
                    wbuf, wkey = R_tm.next()
                    for tt in range(4):
                        b = nextbank([1, 2, 3, 4])
                        for kc in range(8):
                            S.mm(ps[b][:, 0:256], hn[:, kc, tt * 128:(tt + 1) * 128], wbuf[:, kc, :],
                                 kc == 0, kc == 7, r=[wkey, ("hn", kc)], w=pk(b, 0, 256))
                        if g == 0:
                            S.op("act", "copy", gv_sb[:, tt, :], ps[b][:, 0:256], r=pk(b, 0, 256),
                                 w=gv_b.k(tt * 256, tt * 256 + 256))
                        elif g == 1:
                            lo = (tt % 2) * 256
                            e1 = tA[5].ap[:, lo:lo + 256]
                            k1 = tA[5].k(lo, lo + 256)
                            kg = gsg_b.k(tt * 256, tt * 256 + 256)
                            S.op("act", "activation", e1, ps[b][:, 0:256], AF.Exp, scale=-1.0,
                                 r=pk(b, 0, 256), w=k1)
                            recip1p(e1, k1)
                            S.op("dve", "tensor_tensor", gsg[:, tt, :], ps[b][:, 0:256], e1, ALU.mult,
                                 r=pk(b, 0, 256) + k1, w=kg)
                            S.op("dve", "tensor_tensor", gsg[:, tt, :], gsg[:, tt, :], g8[:], ALU.mult,
                                 r=kg + ["g8"], w=kg)
                        else:
                            half = g - 2
                            eng = "act" if tt % 2 == 0 else "dve"
                            meth = "copy" if eng == "act" else "tensor_copy"
                            S.op(eng, meth, vS[:, 4 * I + tt, half * 256:(half + 1) * 256],
                                 ps[b][:, 0:256], r=pk(b, 0, 256), w=[("vS", 4 * I + tt, half)])

                ck("tm")
                ck("gla_b")
                trb = psbf(6)

                def emit_kt_transposes():
                    for c in range(4):
                        tr_op(trb[:, c * 128:(c + 1) * 128], kt.ap[:, c * 128:(c + 1) * 128],
                              kt.k() + ["c_ident"], pk(6, 0, 256))
                    S.op("act", "copy", kt_b.ap, trb[:, 0:512], r=pk(6, 0, 256), w=kt_b.k())
                def emit_diag(cc):
                    for w_ in range(31):
                        S.op("dve", "tensor_scalar", dg[:, w_, :], ident[:],
                             pcol_sb[:, PC_CW + cc * 31 + w_:PC_CW + cc * 31 + w_ + 1], None, ALU.mult,
                             r=["c_ident", "pcol"], w=dg_b.k(w_ * 128, w_ * 128 + 128))

                def emit_conv(cc):
                    by = nextbank([2, 3])
                    for w_ in range(31):
                        S.mm(ps[by][:], dg[:, w_, :], zb[:, cc, w_:w_ + T], w_ == 0, w_ == 30,
                             r=dg_b.k(w_ * 128, w_ * 128 + 128) + [("zb", cc)], w=pk(by))
                    S.op("dve", "tensor_copy", zb[:, cc, 0:30], zb[:, cc, T:T + 30], r=[("zb", cc)],
                         w=[("zb", cc)])
                    ysb = (gq_sb, gk_sb)[cc]
                    ybf = tB[2 + cc]
                    cb = pcol_sb[:, PC_CB + cc:PC_CB + cc + 1]
                    S.op("act", "activation", ysb.ap, ps[by][:], AF.Identity, bias=cb,
                         r=pk(by) + ["pcol"], w=ysb.k())
                    S.op("act", "activation", ybf.ap, ps[by][:], AF.Identity, bias=cb,
                         r=pk(by) + ["pcol"], w=ybf.k())

                    def c2():
                        bm = nextbank([2, 3])
                        S.mm(ps[bm][:], blk64[:], ybf.ap, True, True, r=ybf.k() + ["c_blk64"], w=pk(bm))
                        S.op("dve", "tensor_tensor", ysb.ap, ysb.ap, ps[bm][:], ALU.subtract,
                             r=ysb.k() + pk(bm), w=ysb.k())
                        S.op("dve", "tensor_tensor", ybf.ap, ysb.ap, ysb.ap, ALU.mult, r=ysb.k(),
                             w=ybf.k())

                        def c3():
                            bv = nextbank([2, 3])
                            S.mm(ps[bv][:], blk64[:], ybf.ap, True, True, r=ybf.k() + ["c_blk64"],
                                 w=pk(bv))
                            rs = tA[5]
                            rstd_act(rs.ap, rs.k(), ps[bv][:], pk(bv), EPS)
                            S.op("dve", "tensor_tensor", ysb.ap, ysb.ap, rs.ap, ALU.mult,
                                 r=ysb.k() + rs.k(), w=ysb.k())
                            S.op("dve", "tensor_scalar", ysb.ap, ysb.ap,
                                 pcol_sb[:, PC_CG + cc:PC_CG + cc + 1],
                                 pcol_sb[:, PC_CBETA + cc:PC_CBETA + cc + 1], ALU.mult, ALU.add,
                                 r=ysb.k() + ["pcol"], w=ysb.k())
                            S.op("act", "activation", rs.ap, ysb.ap, AF.Exp, scale=-1.0, r=ysb.k(),
                                 w=rs.k())
                            recip1p(rs.ap, rs.k())
                            S.op("dve", "tensor_tensor", mixT[:, 2 + cc, :], ysb.ap, rs.ap, ALU.mult,
                                 r=ysb.k() + rs.k(), w=mixT_b.k((2 + cc) * T, (3 + cc) * T))
                            return None
                        return c3
                    return c2

                ck("gla_c")

                def emit_gla_scores(c):
                    cs_ = slice(c * 128, (c + 1) * 128)
                    S.mm(ps[7][:], kt.ap[:, cs_], Qblk[:, c, :, :].rearrange("p h i -> p (h i)"), True, True,
                         r=kt.k() + ["Qblk"], w=pk(7))
                    scm = glr_bf
                    S.op("dve", "tensor_tensor", scm.ap.rearrange("p (h i) -> p h i", i=128),
                         ps[7][:].rearrange("p (h i) -> p h i", i=128),
                         tri[:].unsqueeze(1).broadcast_to([128, 4, 128]), ALU.mult,
                         r=pk(7) + ["c_tri"], w=scm.k())

                def emit_gla_chunk(c):
                    cs_ = slice(c * 128, (c + 1) * 128)
                    kgv = gv_b.k(c * 256, c * 256 + 256)
                    scm = glr_bf
                    og = og2[c % 2]
                    ck("gla_d")
                    for h in range(4):
                        S.mm(ps[1][32 * h:32 * h + 32, 0:64], kt_tok[:, c, 32 * h:32 * h + 32],
                             gv_sb[:, c, 64 * h:64 * h + 64], True, True,
                             r=kt_b.k() + kgv, w=pk(1), tp=(0, 32 * h))
                    ck("gla_e")
                    for h in range(4):
                        S.mm(ps[0][:, 64 * h:64 * h + 64], scm.ap[:, h * 128:(h + 1) * 128],
                             gv_sb[:, c, 64 * h:64 * h + 64], h == 0, False, r=scm.k() + kgv,
                             w=pk(0, 0, 256), skip=True)
                    S.mm(ps[0][:, 0:256], qt.ap[:, cs_], Sblk[:], False, True, r=qt.k() + ["Sblk"],
                         w=pk(0, 0, 256), skip=True)
                    ck("gla_f")
                    ebl = bufC.ap[:, c * 128 + 127:c * 128 + 128]
                    U = small.ap[:, 0:64]
                    S.op("dve", "tensor_tensor", U, ps[1][:, 0:64], S32[:], ALU.add,
                         r=pk(1) + ["S32"], w=small.k())
                    S.op("dve", "tensor_scalar", S32[:], U, ebl, None, ALU.mult, r=small.k() + bufC.k(),
                         w=["S32"])
                    for h in range(4):
                        S.op("dve", "tensor_scalar", Sblk[32 * h:32 * h + 32, 64 * h:64 * h + 64],
                             U[32 * h:32 * h + 32, :], ebl[32 * h:32 * h + 32, :], None, ALU.mult,
                             r=small.k() + bufC.k(), w=["Sblk"])
                    ck("gla_g")
                    osq_ap = mixT_b.ap[:, 7 * T:8 * T].bitcast(F32)
                    osq_k = mixT_b.k(7 * T, 8 * T)
                    osb_ap = mixT_b.ap[:, 6 * T:7 * T].bitcast(F32)
                    osb_k = mixT_b.k(6 * T, 7 * T)
                    S.op("act", "copy", osb_ap, ps[0][:, 0:256], r=pk(0, 0, 256), w=osb_k)
                    S.op("dve", "tensor_tensor", osq_ap, osb_ap, osb_ap, ALU.mult, r=osb_k, w=osq_k)
                    ss = small.ap[:, 64:68]
                    S.op("dve", "tensor_reduce", ss, osq_ap.rearrange("p (h v) -> p h v", v=64),
                         AX.X, ALU.add, r=osq_k, w=small.k())
                    rstd_act(ss, small.k(), ss, small.k(), 64.0 * EPS)
                    S.op("dve", "tensor_tensor", osq_ap.rearrange("p (h v) -> p h v", v=64),
                         osb_ap.rearrange("p (h v) -> p h v", v=64),
                         ss.unsqueeze(2).broadcast_to([128, 4, 64]), ALU.mult,
                         r=osb_k + small.k(), w=osq_k)
                    S.op("dve", "tensor_tensor", og.ap, osq_ap, gsg[:, c, :], ALU.mult,
                         r=osq_k + gsg_b.k(c * 256, c * 256 + 256), w=og.k())
                    ck("gla_h")

                    def gla_b():
                        for hh in range(2):
                            tr_op(trb[:, 512 + hh * 128:512 + (hh + 1) * 128],
                                  og.ap[:, hh * 128:(hh + 1) * 128], og.k() + ["c_ident"], pk(6, 256, 384))
                        S.op("dve", "tensor_copy", mixT[:, 0:2, cs_],
                             trb[:, 512:768].rearrange("p (a b) -> p a b", b=128),
                             r=pk(6, 256, 384), w=mixT_b.k(0, 2 * T))
                        return None
                    return gla_b

                nkb = 4 * I + 4

                def emit_head(h):
                    kqn = qn_b.k(h * T, (h + 1) * T)

                    def emit_S(j):
                        q0 = 0 if j < 4 * I else (j - 4 * I) * 128
                        for m in range(2):
                            bS = 2 * (j % 2) + m
                            S.mm(ps[bS][:, q0:T], kT[64 * m:64 * m + 64, h, j * 128:(j + 1) * 128],
                                 qn[64 * m:64 * m + 64, h, q0:T], True, True,
                                 r=[("kT", h, j // 4)] + kqn, w=pk(bS, q0, T), tp=(64 * m, 0))

                    def emit_exp(j):
                        q0 = 0 if j < 4 * I else (j - 4 * I) * 128
                        p = j % 2
                        ptb = PTp[p]
                        src = psall[:, (2 * p) * 512:(2 * p + 2) * 512].rearrange("p (m q) -> p m q", q=512)
                        dstv = ptb.ap.rearrange("p (m q) -> p m q", q=512)
                        S.op("act", "activation", dstv[:, :, q0:T], src[:, :, q0:T], AF.Exp, scale=0.125,
                             r=pk(2 * p) + pk(2 * p + 1), w=ptb.k())
                        if j >= 4 * I:
                            S.op("dve", "tensor_tensor", dstv[:, :, q0:q0 + 128], dstv[:, :, q0:q0 + 128],
                                 tri[:].unsqueeze(1).broadcast_to([128, 2, 128]), ALU.mult,
                                 r=ptb.k() + ["c_tri"], w=ptb.k())

                    def emit_AV(j):
                        diag = j >= 4 * I
                        q0 = 0 if not diag else (j - 4 * I) * 128
                        vv = vS[:, j, h * 128:(h + 1) * 128]
                        vk = ("vS", j, h // 2)
                        for m in range(2):
                            ptb = PTp[j % 2]
                            ptap = ptb.ap[:, m * T:(m + 1) * T]
                            if not diag:
                                regions = [(0, T, False)]
                            else:
                                regions = [(q0, q0 + 128, True)]
                                if q0 + 128 < T:
                                    regions.append((q0 + 128, T, False))
                            for ri, (a, b_, last) in enumerate(regions):
                                S.mm(ps[4 + m][:, a:b_], vv, ptap[:, a:b_], j == 0 and ri == 0, last,
                                     r=[vk] + ptb.k(), w=pk(4 + m, a, b_), skip=True)
                            for ri, (a, b_, last) in enumerate(regions):
                                S.mm(ps[6 + m][:, a:b_], ones1[:], ptap[:, a:b_], j == 0 and ri == 0,
                                     last, r=["c_ones1"] + ptb.k(), w=pk(6 + m, a, b_), skip=True)

                    emit_S(0)
                    for j in range(nkb):
                        if j + 1 < nkb:
                            emit_S(j + 1)
                        emit_exp(j)
                        emit_AV(j)
                    r0, r1, o0, o1 = tA[1], tA[2], tA[3 + (h % 2)], tA[5]
                    S.op("act", "activation", r0.ap, ps[6][:], AF.Ln, r=pk(6), w=r0.k())
                    S.op("dve", "tensor_copy", o0.ap, ps[4][:], r=pk(4), w=o0.k())
                    S.op("act", "activation", r1.ap, ps[7][:], AF.Ln, r=pk(7), w=r1.k())
                    S.op("dve", "tensor_copy", o1.ap, ps[5][:], r=pk(5), w=o1.k())
                    osqb = tB[h % 2]

                    def post1b():
                        return _post1b(h, r0, r1, o0, o1, osqb)
                    return post1b

                def _post1b(h, r0, r1, o0, o1, osqb):
                    S.op("act", "activation", r0.ap, r0.ap, AF.Exp, scale=-1.0, r=r0.k(), w=r0.k())
                    S.op("act", "activation", r1.ap, r1.ap, AF.Exp, scale=-1.0, r=r1.k(), w=r1.k())
                    S.op("dve", "tensor_tensor", o0.ap, o0.ap, r0.ap, ALU.mult, r=o0.k() + r0.k(),
                         w=o0.k())
                    S.op("dve", "scalar_tensor_tensor", o1.ap, o1.ap, der[:, 2:3], r1.ap, ALU.mult,
                         ALU.mult, r=o1.k() + r1.k() + ["der"], w=o1.k())
                    S.op("dve", "tensor_tensor", o0.ap, o0.ap, o1.ap, ALU.add, r=o0.k() + o1.k(),
                         w=o0.k())
                    S.op("dve", "tensor_tensor", osqb.ap, o0.ap, o0.ap, ALU.mult, r=o0.k(), w=osqb.k())

                    def head_fin():
                        bs_ = nextbank([2, 3])
                        S.mm(ps[bs_][:], ones128[:], osqb.ap, True, True, r=osqb.k() + ["c_ones128"],
                             w=pk(bs_))
                        rstd_act(r0.ap, r0.k(), ps[bs_][:], pk(bs_), EPS)
                        S.op("dve", "scalar_tensor_tensor", mixT[:, 4 + h, :], o0.ap, der[:, 1:2], r0.ap,
                             ALU.mult, ALU.mult, r=o0.k() + r0.k() + ["der"],
                             w=mixT_b.k((4 + h) * T, (5 + h) * T))
                        return None
                    return head_fin

                pending = []
                for c in range(4):
                    emit_gla_scores(c)
                    if c < 2:
                        emit_diag(c)
                    p1b = emit_head(c)
                    if c == 0:
                        emit_kt_transposes()
                    glb = emit_gla_chunk(c)
                    fin = p1b()
                    cv = emit_conv(c) if c < 2 else None
                    nxt = [f() for f in pending]
                    pending = [fin, glb] + [f for f in nxt if f is not None]
                    if cv is not None:
                        pending.append(cv)
                while pending:
                    pending = [g for g in (f() for f in pending) if g is not None]
                ck("gla")
                ck("conv")
                dbg("mix%d_%d" % (l, I), mixT, [128, 8, T], BF16, r=mixT_b.k())

                ck("attn")
                for j in range(8):
                    wbuf, wkey = R_fm.next()
                    b = nextbank([0, 1])
                    for ki, kc in enumerate((4, 5, 6, 2, 3, 7, 0, 1)):
                        S.mm(ps[b][:], wbuf[:, kc, :], mixT[:, kc, :], ki == 0, ki == 7,
                             r=[wkey] + mixT_b.k(kc * T, (kc + 1) * T), w=pk(b))
                    S.op("dve", "tensor_tensor", xc(j), xc(j), ps[b][:], ALU.add,
                         r=[xk(j)] + pk(b), w=[xk(j)])
                    norm_sq(j)
                    if j >= 2:
                        norm_mm(j - 2, 2, j == 2, False)
                norm_mm(6, 2, False, False)
                norm_mm(7, 2, False, True)

                for c_ in range(8):
                    dbg("xa%d_%d_%d" % (l, I, c_), xc(c_), [128, T], F32, r=[xk(c_)])

                ck("oproj")
                rmsnorm(PC_FFNG, 2, True, stats_done=True)
                rtv = rt.rearrange("p (a b) -> p a b", b=80)
                lg = rtv[:, 0, :].rearrange("p (t n) -> p t n", n=20)
                S.op("dve", "tensor_tensor", lg, ps[3][:, 0:80].rearrange("p (t n) -> p t n", n=20),
                     prow_sb[:, PR_RB:PR_RB + 80].rearrange("p (t n) -> p t n", n=20), ALU.add,
                     r=pk(3, 0, 128) + ["prow"], w=rt_b.k())
                RT = dict(r=rt_b.k(), w=rt_b.k())
                gl = lg[:, :, 0:4]
                gmax = rtv[:, 1, 0:4]
                goh = rtv[:, 1, 4:20].rearrange("p (t n) -> p t n", n=4)
                gd = rtv[:, 1, 20:36].rearrange("p (t n) -> p t n", n=4)
                gsum = rtv[:, 1, 36:40]
                S.op("dve", "tensor_reduce", gmax, gl, AX.X, ALU.max, **RT)
                S.op("dve", "tensor_tensor", goh, gl, gmax.unsqueeze(2).broadcast_to([128, 4, 4]),
                     ALU.is_equal, **RT)
                S.op("dve", "tensor_tensor", gd, gl, gmax.unsqueeze(2).broadcast_to([128, 4, 4]),
                     ALU.subtract, **RT)
                S.op("act", "activation", gd, gd, AF.Exp, **RT)
                S.op("dve", "tensor_reduce", gsum, gd, AX.X, ALU.add, **RT)
                S.op("dve", "reciprocal", gsum, gsum, **RT)
                el = lg[:, :, 4:20].rearrange("p t (g e) -> p t g e", e=4)
                tmp4 = rtv[:, 2, 0:64].rearrange("p (t g e) -> p t g e", g=4, e=4)
                S.op("dve", "tensor_tensor", tmp4, el, goh.unsqueeze(3).broadcast_to([128, 4, 4, 4]),
                     ALU.mult, **RT)
                sel = rtv[:, 3, 0:16].rearrange("p (t e) -> p t e", e=4)
                S.op("dve", "tensor_reduce", sel, tmp4.rearrange("p t g e -> p t e g"), AX.X, ALU.add,
                     **RT)
                m1 = rtv[:, 3, 16:20]
                mk1 = rtv[:, 3, 20:36].rearrange("p (t e) -> p t e", e=4)
                sel2 = rtv[:, 3, 36:52].rearrange("p (t e) -> p t e", e=4)
                m2 = rtv[:, 3, 52:56]
                mk2 = rtv[:, 3, 56:72].rearrange("p (t e) -> p t e", e=4)
                S.op("dve", "tensor_reduce", m1, sel, AX.X, ALU.max, **RT)
                S.op("dve", "tensor_tensor", mk1, sel, m1.unsqueeze(2).broadcast_to([128, 4, 4]),
                     ALU.is_equal, **RT)
                S.op("dve", "scalar_tensor_tensor", sel2, mk1, -1e30, sel, ALU.mult, ALU.add, **RT)
                S.op("dve", "tensor_reduce", m2, sel2, AX.X, ALU.max, **RT)
                S.op("dve", "tensor_tensor", mk2, sel2, m2.unsqueeze(2).broadcast_to([128, 4, 4]),
                     ALU.is_equal, **RT)
                dd = rtv[:, 4, 0:4]
                w1 = rtv[:, 4, 4:8]
                w2 = rtv[:, 4, 8:12]
                S.op("dve", "tensor_tensor", dd, m2, m1, ALU.subtract, **RT)
                S.op("act", "activation", dd, dd, AF.Exp, **RT)
                S.op("dve", "tensor_scalar", w1, dd, 1.0, None, ALU.add, **RT)
                S.op("dve", "reciprocal", w1, w1, **RT)
                S.op("dve", "tensor_tensor", w2, dd, w1, ALU.mult, **RT)
                S.op("dve", "tensor_tensor", w1, w1, gsum, ALU.mult, **RT)
                S.op("dve", "tensor_tensor", w2, w2, gsum, ALU.mult, **RT)
                wl = rtv[:, 4, 12:28].rearrange("p (t e) -> p t e", e=4)
                wl2 = rtv[:, 4, 28:44].rearrange("p (t e) -> p t e", e=4)
                S.op("dve", "tensor_tensor", wl, mk1, w1.unsqueeze(2).broadcast_to([128, 4, 4]),
                     ALU.mult, **RT)
                S.op("dve", "tensor_tensor", wl2, mk2, w2.unsqueeze(2).broadcast_to([128, 4, 4]),
                     ALU.mult, **RT)
                S.op("dve", "tensor_tensor", wl, wl, wl2, ALU.add, **RT)
                gates = rtv[:, 5, 0:64].rearrange("p (t g e) -> p t g e", g=4, e=4)
                S.op("dve", "tensor_tensor", gates, goh.unsqueeze(3).broadcast_to([128, 4, 4, 4]),
                     wl.unsqueeze(2).broadcast_to([128, 4, 4, 4]), ALU.mult, **RT)
                gates_te = rtv[:, 5, 0:64].rearrange("p (t n) -> p t n", n=16)
                dbg("gates%d_%d" % (l, I), rtv[:, 5, 0:64], [128, 64], F32, r=rt_b.k())

                ck("router")
                def emit_G(e):
                    De_e = De[e % 2]
                    S.op("dve", "tensor_tensor", De_e.ap.rearrange("p (c t) -> p c t", t=128),
                         ident[:].unsqueeze(1).broadcast_to([128, 4, 128]),
                         gates_te[:, :, e:e + 1].broadcast_to([128, 4, 128]), ALU.mult,
                         r=rt_b.k() + ["c_ident"], w=De_e.k())
                    bG = 4 + (e % 2)
                    for tt in range(4):
                        S.mm(ps[bG][:, tt * 128:(tt + 1) * 128], ones1[:],
                             De_e.ap[:, tt * 128:(tt + 1) * 128], True, True,
                             r=De_e.k() + ["c_ones1"], w=pk(bG, tt * 128, tt * 128 + 128))

                for e in range(NE):
                    bG = 4 + (e % 2)
                    wg_buf, wg_key = R_gu.next()
                    wu_buf, wu_key = R_gu.next()
                    for hc in range(2):
                        par = (e * 2 + hc) % 2
                        bg_, bu_ = 2 * par, 2 * par + 1
                        for kc in range(8):
                            S.mm(ps[bg_][:], wg_buf[:, hc, kc, :], hn[:, kc, :], kc == 0, kc == 7,
                                 r=[wg_key, ("hn", kc)], w=pk(bg_))
                        for kc in range(8):
                            S.mm(ps[bu_][:], wu_buf[:, hc, kc, :], hn[:, kc, :], kc == 0, kc == 7,
                                 r=[wu_key, ("hn", kc)], w=pk(bu_))
                        if hc == 0 and e == 0:
                            emit_G(0)
                        if hc == 1 and e + 1 < NE:
                            emit_G(e + 1)
                        S.op("act", "activation", sgb[par].ap, ps[bg_][:], AF.Silu, r=pk(bg_),
                             w=sgb[par].k())
                        S.op("dve", "tensor_tensor", hb[par].ap, sgb[par].ap, ps[bu_][:], ALU.mult,
                             r=sgb[par].k() + pk(bu_), w=hb[par].k())
                        eh = e * 2 + hc
                        S.op("dve", "tensor_tensor", actp[:, eh, :], hb[par].ap, ps[bG][:], ALU.mult,
                             r=hb[par].k() + pk(bG), w=actp_b.k(eh * T, (eh + 1) * T))
                for j in DOWN_ORDER:
                    by = 6 + (j % 2)
                    for half in range(2):
                        wbuf, wkey = R_wd.next()
                        for i in range(16):
                            eh = half * 16 + i
                            S.mm(ps[by][:], wbuf[:, i, :], actp[:, eh, :], eh == 0, eh == 31,
                                 r=[wkey] + actp_b.k(eh * T, (eh + 1) * T), w=pk(by))
                    if j < 6:
                        S.op("dve", "tensor_tensor", xc(j), xc(j), ps[by][:], ALU.add,
                             r=[xk(j)] + pk(by), w=[xk(j)])
                        S.dma("pool", dst[j * 128:(j + 1) * 128, t0:t0 + T], xc(j), r=[xk(j)],
                              w=[("xres", l + 1, I, j)])
                    else:
                        tmp = xn32[j - 6]
                        S.op("dve", "tensor_tensor", tmp.ap, xc(j), ps[by][:], ALU.add,
                             r=[xk(j)] + pk(by), w=tmp.k())
                        S.dma("pool", dst[j * 128:(j + 1) * 128, t0:t0 + T], tmp.ap, r=tmp.k(),
                              w=[("xres", l + 1, I, j)])
                    if I + 1 < NB:
                        nl, nI, nsrc = l, I + 1, src
                    elif l + 1 < L:
                        nl, nI, nsrc = l + 1, 0, xmid
                    else:
                        nl = None
                    if nl is not None:
                        nj = []
                        if j >= 2:
                            nj = [j - 2]
                        if j == 2:
                            nj.append(6)
                        if j == 3:
                            nj.append(7)
                        for jn in nj:
                            S.dma("pool", xc(jn, gb + 1), nsrc[jn * 128:(jn + 1) * 128, nI * T:(nI + 1) * T],
                                  r=[("xres", nl, nI, jn)] if nl > 0 else [], w=[xk(jn, gb + 1)])
                ck("moe")

        except _Stop:
            pass
        S.add("sp", None, reads=[("xres", L, I, j) for I in range(NB) for j in range(8)] +
              [("dbg", n) for n in dbg_outs] +
              [("c_wout", l_, 7) for l_ in range(L)] + [("c_wd", l_, 1) for l_ in range(L)])
        S.finalize(st)
        info = dict(ops=S.n_ops, counts=S.max_counts, arena=(mixer_top, moe_top), sbuf_left=nc.sbuf_bytes_remaining,
                    pe_tags=[o.tag for o in S.ops if o.stream == "pe" and o.fn is not None],
                    act_tags=[o.tag for o in S.ops if o.stream == "act" and o.fn is not None and not o.is_dma],
                    dve_tags=[o.tag for o in S.ops if o.stream == "dve" and o.fn is not None and not o.is_dma])
    return nc, info


def prep_weights(inp, L):
    f = np.float32
    w_in = np.asarray(inp["w_in"], f)
    out = {}

    def chunks_kp(w, cols, width):
        res = np.stack([w[:, c:c + width] for c in cols], 0)
        res = res.reshape(len(cols), 8, 128, width).transpose(0, 2, 1, 3)
        return np.ascontiguousarray(res)

    out["w_fm"] = np.stack([chunks_kp(w_in[l], FM_COLS, 128).reshape(14 * 128, 1024) for l in range(L)])
    out["w_glr"] = np.stack([chunks_kp(w_in[l], [512], 16).reshape(128, 128) for l in range(L)])
    out["w_tm"] = np.stack([chunks_kp(w_in[l], TM_COLS, 256).reshape(4 * 128, 2048) for l in range(L)])
    w_o = np.asarray(inp["w_out"], f)
    out["w_out"] = np.stack([chunks_kp(w_o[l], [128 * j for j in range(8)], 128).reshape(8 * 128, 1024)
                             for l in range(L)])
    wg = np.asarray(inp["expert_w_gate"], f)
    wu = np.asarray(inp["expert_w_up"], f)
    wdn = np.asarray(inp["expert_w_down"], f)
    gu = np.stack([wg, wu], 2)
    gu = gu.reshape(L, NE, 2, 8, 128, 2, 128)
    gu = gu.transpose(0, 1, 2, 4, 5, 3, 6)
    out["w_gu"] = np.ascontiguousarray(gu).reshape(L, 32 * 128, 2048)
    wd = wdn.reshape(L, 2, 8, 2, 128, 8, 128)
    wd = wd.transpose(0, 5, 1, 4, 2, 3, 6)
    out["w_d"] = np.ascontiguousarray(wd).reshape(L, 16 * 128, 2048)
    wr = np.concatenate([np.asarray(inp["router_group_w"], f), np.asarray(inp["router_expert_w"], f)], -1)
    wr = wr.reshape(L, 8, 128, 20).transpose(0, 2, 1, 3)
    out["w_r"] = np.ascontiguousarray(wr).reshape(L, 128, 160)
    out["w_gate"] = np.ascontiguousarray(np.asarray(inp["gla_gate_w"], f))
    pc = np.zeros((L, 128, NPC), f)
    pr = np.zeros((L, 128, NPR), f)
    for l in range(L):
        pc[l, :, PC_MIXG:PC_MIXG + 8] = np.asarray(inp["mix_norm_g"][l], f).reshape(8, 128).T
        pc[l, :, PC_FFNG:PC_FFNG + 8] = np.asarray(inp["ffn_norm_g"][l], f).reshape(8, 128).T
        pc[l, :, PC_GB] = np.asarray(inp["gla_gate_b"][l], f)
        pc[l, :, PC_CB:PC_CB + 2] = np.asarray(inp["conv_b"][l], f).reshape(2, 128).T
        pc[l, :, PC_CG:PC_CG + 2] = np.asarray(inp["conv_norm_g"][l], f).reshape(2, 128).T
        pc[l, :, PC_CBETA:PC_CBETA + 2] = np.asarray(inp["conv_norm_b"][l], f).reshape(2, 128).T
        pc[l, :, PC_QG] = np.tile(np.asarray(inp["diff_qnorm_g"][l], f), 2)
        pc[l, :, PC_KG] = np.tile(np.asarray(inp["diff_knorm_g"][l], f), 2)
        pc[l, :, PC_SUB] = np.asarray(inp["diff_subln_g"][l], f)
        cw = np.asarray(inp["conv_w"][l], f)
        pc[l, :, PC_CW:PC_CW + 62] = cw.reshape(31, 2, 128).transpose(2, 1, 0).reshape(128, 62)
        pr[l, :, PR_G8:PR_G8 + 256] = np.tile(np.asarray(inp["gla_norm_g"][l], f), 4)[None, :]
        rb = np.concatenate([np.asarray(inp["router_group_b"][l], f), np.asarray(inp["router_expert_b"][l], f)])
        pr[l, :, PR_RB:PR_RB + 80] = np.tile(rb, 4)[None, :]
        lq = np.concatenate([np.asarray(inp[k][l], f) for k in ("diff_lq1", "diff_lk1", "diff_lq2", "diff_lk2")])
        pr[l, :, PR_LQ:PR_LQ + 256] = lq[None, :]
    out["pcol"] = pc
    out["prow"] = pr
    return out


_CACHE = {}


def run(inputs, SEQ, DEPTH, n_cores, debug=(), trace=False, stop=None):
    key = (SEQ, DEPTH, tuple(debug), stop)
    if key not in _CACHE:
        _CACHE[key] = build(SEQ, DEPTH, debug, stop)
    nc, info = _CACHE[key]
    wts = prep_weights(inputs, DEPTH)
    x = np.asarray(inputs["x"], np.float32)
    in_maps = []
    for c in range(n_cores):
        m = dict(wts)
        m["xT"] = np.ascontiguousarray(x[c].T)
        in_maps.append(m)
    res = run_bass_kernel_spmd(nc, in_maps, core_ids=list(range(n_cores)), trace=trace)
    y = np.stack([np.asarray(r["yT"]).T for r in res.results], 0)
    return np.ascontiguousarray(y.astype(np.float32)), res, info


def kernel(**inputs):
    x = np.asarray(inputs["x"])
    B, SEQ, _ = x.shape
    DEPTH = np.asarray(inputs["w_in"]).shape[0]
    y, _, _ = run(inputs, SEQ, DEPTH, B)
    return y
```
